# Optimizing a Trainium2 kernel written in Bass

```python
import jax
import jax.numpy as jnp
from jax import lax
import numpy as np

D_MODEL = 1024
BATCH = 8
SEQ = 2048
DEPTH = 4

GRID_W = 64
CTX_LEN = 256
N_EVEN = (DEPTH + 1) // 2
N_ODD = DEPTH // 2
N_MOD = 6
EPS = 1e-6
NEG_BIG = -1e9
LB_MIN = 1e-30

HG_WIDTH = D_MODEL // 2
HG_HEAD_DIM = 128
HG_HEADS = HG_WIDTH // HG_HEAD_DIM
HG_CHUNK = 64
RG_WIDTH = D_MODEL - HG_WIDTH
RG_BLOCKS = 8
RG_BLOCK = RG_WIDTH // RG_BLOCKS
RG_CONV = 4
RG_C = 8.0
EVEN_SPLITS = (HG_WIDTH, HG_WIDTH, HG_WIDTH, HG_WIDTH, HG_WIDTH, RG_WIDTH, RG_WIDTH)
EVEN_IN = 5 * HG_WIDTH + 2 * RG_WIDTH
NA_HEADS = 16
NA_HEAD_DIM = D_MODEL // NA_HEADS
NA_KR = 8
NA_KC = 16
NA_QB = 16
NA_CB = NA_QB + NA_KC
D_FF = 2816
N_EXPERTS = 8
TOP_K = 2
D_FF_EXPERT = 2816

kernel_name = "hybrid_hgrn2_rglru_natten_moe_dit"


def _rmsnorm(x, g):
    xf = x.astype(jnp.float32)
    y = xf * lax.rsqrt(jnp.mean(xf * xf, axis=-1, keepdims=True) + EPS)
    return (y * g.astype(jnp.float32)).astype(x.dtype)


def _modulate(x, g, shift, scale):
    return _rmsnorm(x, g) * (1 + scale) + shift


def _adaln(cond, w_mod, b_mod):
    return jnp.split(jax.nn.silu(cond) @ w_mod + b_mod, N_MOD, axis=-1)


def _swiglu(x, w1, w3, w2):
    return (jax.nn.silu(x @ w1) * (x @ w3)) @ w2


def _moe(x, w_router, w1, w3, w2):
    logits = (x @ w_router).astype(jnp.float32)
    top_v, top_i = lax.top_k(logits, TOP_K)
    top_w = jax.nn.softmax(top_v, axis=-1)
    gates = jnp.einsum('btk,btke->bte', top_w,
                       jax.nn.one_hot(top_i, N_EXPERTS, dtype=jnp.float32)).astype(x.dtype)
    y = gates[..., 0:1] * _swiglu(x, w1[0], w3[0], w2[0])
    for e in range(1, N_EXPERTS):
        y = y + gates[..., e:e + 1] * _swiglu(x, w1[e], w3[e], w2[e])
    return y


def _flip(a):
    return jnp.flip(a, axis=1)


def _prefix_scan(scan_fn, ctx_in, lat_in, reverse):
    if reverse:
        ctx_in = [_flip(a) for a in ctx_in]
        lat_in = [_flip(a) for a in lat_in]
    o_c, s_c = scan_fn(*ctx_in, None)
    o_l, _ = scan_fn(*lat_in, s_c)
    if reverse:
        o_c, o_l = _flip(o_c), _flip(o_l)
    return o_c, o_l


def _gla_chunk_scan(q, k, v, logf, s0):
    B, T, H, K = q.shape
    V = v.shape[-1]
    n = T // HG_CHUNK
    if s0 is None:
        s0 = jnp.zeros((B, H, K, V), jnp.float32)

    def to_chunks(a):
        return a.reshape(B, n, HG_CHUNK, H, a.shape[-1]).transpose(1, 0, 3, 2, 4)

    tri = np.tril(np.ones((HG_CHUNK, HG_CHUNK), dtype=bool))[:, :, None]

    def step(s, inp):
        qc, kc, vc, gc = inp
        b = jnp.cumsum(gc, axis=2)
        o_inter = jnp.einsum('bhtk,bhkv->bhtv', qc * jnp.exp(b), s)
        diff = jnp.where(tri, b[:, :, :, None, :] - b[:, :, None, :, :], NEG_BIG)
        attn = jnp.einsum('bhtk,bhtsk,bhsk->bhts', qc, jnp.exp(diff), kc)
        o = o_inter + jnp.einsum('bhts,bhsv->bhtv', attn, vc)
        b_last = b[:, :, -1:, :]
        s_new = (jnp.exp(b_last[:, :, 0, :])[..., None] * s
                 + jnp.einsum('bhsk,bhsv->bhkv', kc * jnp.exp(b_last - b), vc))
        return s_new, o

    s_fin, o = lax.scan(step, s0, (to_chunks(q), to_chunks(k), to_chunks(v), to_chunks(logf)))
    return o.transpose(1, 0, 3, 2, 4).reshape(B, T, H, V), s_fin


def _linear_scan(a, b, h0):
    if h0 is not None:
        b = b.at[:, 0].add(a[:, 0] * h0)
    _, h = lax.associative_scan(lambda l, r: (l[0] * r[0], r[0] * l[1] + r[1]), (a, b), axis=1)
    return h, h[:, -1]


def _hg_heads(a):
    return a.astype(jnp.float32).reshape(a.shape[:2] + (HG_HEADS, HG_HEAD_DIM))


def _hg_gates(z, lb):
    z = z.astype(jnp.float32)
    k = (1 - lb) * jax.nn.sigmoid(-z)
    logf = jnp.logaddexp(jnp.log(jnp.maximum(lb, LB_MIN)), jnp.log1p(-lb) + jax.nn.log_sigmoid(z))
    return _hg_heads(k), _hg_heads(logf)


def _dwconv_centred(x, w, b):
    left = RG_CONV // 2
    y = lax.conv_general_dilated(x, w[:, None, :], window_strides=(1,),
                                 padding=[(left, RG_CONV - 1 - left)],
                                 dimension_numbers=('NWC', 'WIO', 'NWC'),
                                 feature_group_count=x.shape[-1])
    return y + b


def _blockdiag(x, w, b):
    xb = x.reshape(x.shape[:-1] + (RG_BLOCKS, RG_BLOCK))
    return jnp.einsum('btni,nij->btnj', xb, w).reshape(x.shape) + b


def _rglru_coeffs(xc, wa, ba, wx, bx, lam):
    xf = xc.astype(jnp.float32)
    r = jax.nn.sigmoid(_blockdiag(xf, wa, ba))
    ig = jax.nn.sigmoid(_blockdiag(xf, wx, bx))
    log_a = RG_C * r * jax.nn.log_sigmoid(lam.astype(jnp.float32))
    return jnp.exp(log_a), jnp.sqrt(jnp.maximum(-jnp.expm1(2 * log_a), 0.0)) * (ig * xf)


def _even_mixer(u_ctx, u_lat, w_in, w_out, lb, onorm_g, conv_w, conv_b,
                wa, ba, wx, bx, lam, with_ctx_out):
    f32 = jnp.float32
    cuts = [int(v) for v in np.cumsum(EVEN_SPLITS)[:-1]]
    pc = jnp.split(u_ctx @ w_in, cuts, axis=-1)
    pl = jnp.split(u_lat @ w_in, cuts, axis=-1)
    hq = [_hg_heads(jax.nn.silu(p[0])) for p in (pc, pl)]
    hv = [_hg_heads(p[3]) for p in (pc, pl)]
    xr = [_dwconv_centred(p[5], conv_w, conv_b) for p in (pc, pl)]
    hg_c = hg_l = rg_c = rg_l = 0.0
    for d in range(2):
        kc, lfc = _hg_gates(pc[1 + d], lb[d])
        kl, lfl = _hg_gates(pl[1 + d], lb[d])
        o_c, o_l = _prefix_scan(_gla_chunk_scan, (hq[0], kc, hv[0], lfc),
                                (hq[1], kl, hv[1], lfl), d == 1)
        hg_c, hg_l = hg_c + o_c, hg_l + o_l
        ac, bc = _rglru_coeffs(xr[0], wa[d], ba[d], wx[d], bx[d], lam[d])
        al, bl = _rglru_coeffs(xr[1], wa[d], ba[d], wx[d], bx[d], lam[d])
        h_c, h_l = _prefix_scan(_linear_scan, (ac, bc), (al, bl), d == 1)
        rg_c, rg_l = rg_c + h_c, rg_l + h_l

    def merge(hg, rg, p):
        hg = _rmsnorm(hg.reshape(hg.shape[:2] + (HG_WIDTH,)), onorm_g) * jax.nn.silu(p[4].astype(f32))
        rg = rg * jax.nn.gelu(p[6].astype(f32))
        return jnp.concatenate([hg, rg], axis=-1).astype(p[0].dtype) @ w_out

    o_lat = merge(hg_l, rg_l, pl)
    o_ctx = merge(hg_c, rg_c, pc) if with_ctx_out else None
    return o_lat, o_ctx


def _na_column_tables():
    nb = GRID_W // NA_QB
    c0 = np.arange(nb) * NA_QB
    band = np.clip(c0 - NA_KC // 2, 0, GRID_W - NA_CB)
    col_idx = band[:, None] + np.arange(NA_CB)
    qcol = c0[:, None] + np.arange(NA_QB)
    wstart = np.clip(qcol - NA_KC // 2, 0, GRID_W - NA_KC)
    kcol = col_idx[:, None, :]
    mask = (kcol >= wstart[..., None]) & (kcol < wstart[..., None] + NA_KC)
    off = np.clip(kcol - qcol[..., None] + NA_KC - 1, 0, 2 * NA_KC - 2)
    return col_idx, mask, off


def _na_mixer(u_ctx, u_lat, w_qkv, w_o, q_g, k_g, rpb, with_ctx_out):
    f32 = jnp.float32
    B, S, _ = u_lat.shape
    rows = S // GRID_W
    kr = min(NA_KR, rows)
    nb = GRID_W // NA_QB
    scale = NA_HEAD_DIM ** -0.5

    def qkv(u):
        q, k, v = jnp.split(u @ w_qkv, 3, axis=-1)
        shp = u.shape[:2] + (NA_HEADS, NA_HEAD_DIM)
        return (_rmsnorm(q.reshape(shp), q_g) * scale, _rmsnorm(k.reshape(shp), k_g), v.reshape(shp))

    qc, kc, vc = qkv(u_ctx)
    ql, kl, vl = qkv(u_lat)
    kc_h = kc.transpose(0, 2, 1, 3)
    vc_h = vc.transpose(0, 2, 1, 3)

    def to_grid(a):
        return a.reshape(B, rows, GRID_W, NA_HEADS, NA_HEAD_DIM).transpose(0, 3, 1, 2, 4)

    qg, kg, vg = to_grid(ql), to_grid(kl), to_grid(vl)
    col_idx, col_mask, col_off = _na_column_tables()
    bias = rpb.astype(f32)[:, :, col_off].transpose(0, 2, 3, 1, 4)
    bias = jnp.where(col_mask[:, :, None, :], bias, NEG_BIG)
    n_loc = kr * NA_CB

    def row_block(r):
        rs = jnp.clip(r - kr // 2, 0, rows - kr)
        q_r = lax.dynamic_index_in_dim(qg, r, axis=2, keepdims=False).reshape(
            B, NA_HEADS, nb, NA_QB, NA_HEAD_DIM)
        k_b = lax.dynamic_slice_in_dim(kg, rs, kr, axis=2)[:, :, :, col_idx]
        v_b = lax.dynamic_slice_in_dim(vg, rs, kr, axis=2)[:, :, :, col_idx]
        b_r = jnp.take(bias, rs + jnp.arange(kr) - r + (NA_KR - 1), axis=3)
        s_loc = jnp.einsum('bhnqd,bhrncd->bhnqrc', q_r, k_b).astype(f32) + b_r
        s_ctx = jnp.einsum('bhnqd,bhld->bhnql', q_r, kc_h).astype(f32)
        s = jnp.concatenate([s_loc.reshape(B, NA_HEADS, nb, NA_QB, n_loc), s_ctx], axis=-1)
        p = jax.nn.softmax(s, axis=-1).astype(vg.dtype)
        p_loc = p[..., :n_loc].reshape(B, NA_HEADS, nb, NA_QB, kr, NA_CB)
        o = (jnp.einsum('bhnqrc,bhrncd->bhnqd', p_loc, v_b)
             + jnp.einsum('bhnql,bhld->bhnqd', p[..., n_loc:], vc_h))
        return o.reshape(B, NA_HEADS, GRID_W, NA_HEAD_DIM)

    o = lax.map(row_block, jnp.arange(rows))
    o_lat = o.transpose(1, 0, 3, 2, 4).reshape(B, S, D_MODEL) @ w_o
    o_ctx = None
    if with_ctx_out:
        s = jnp.einsum('blhd,bmhd->bhlm', qc, kc).astype(f32)
        p = jax.nn.softmax(s, axis=-1).astype(vc.dtype)
        o_ctx = jnp.einsum('bhlm,bmhd->blhd', p, vc).reshape(u_ctx.shape) @ w_o
    return o_lat, o_ctx


def setup_inputs(seed: int = 0) -> dict:
    key = jax.random.key(seed)
    keys = iter(jax.random.split(key, 40))
    f32 = jnp.float32
    D = D_MODEL

    def nrm(shape, scale):
        return jax.random.normal(next(keys), shape, f32) * scale

    def gain(shape):
        return 1.0 + nrm(shape, 0.1)

    a0 = jax.random.uniform(next(keys), (N_EVEN, 2, RG_WIDTH), f32, 0.9, 0.999)
    p0 = a0 ** (1.0 / RG_C)
    return {
        "x": nrm((BATCH, SEQ, D), 1.0),
        "c": nrm((BATCH, D), 1.0),
        "ctx": nrm((BATCH, CTX_LEN, D), 1.0),
        "c_ctx": nrm((D,), 1.0),
        "w_mod": nrm((DEPTH, D, N_MOD * D), 0.5 * D ** -0.5),
        "b_mod": nrm((DEPTH, N_MOD * D), 0.05),
        "norm_mix_g": gain((DEPTH, D)),
        "norm_ffn_g": gain((DEPTH, D)),
        "ev_w_in": nrm((N_EVEN, D, EVEN_IN), D ** -0.5),
        "ev_w_out": nrm((N_EVEN, D, D), D ** -0.5),
        "hg_lb_logits": nrm((N_EVEN, 2, HG_WIDTH), 1.0),
        "hg_onorm_g": gain((N_EVEN, HG_WIDTH)),
        "rg_conv_w": nrm((N_EVEN, RG_CONV, RG_WIDTH), RG_CONV ** -0.5),
        "rg_conv_b": nrm((N_EVEN, RG_WIDTH), 0.1),
        "rg_wa": nrm((N_EVEN, 2, RG_BLOCKS, RG_BLOCK, RG_BLOCK), RG_BLOCK ** -0.5),
        "rg_ba": nrm((N_EVEN, 2, RG_WIDTH), 0.1),
        "rg_wx": nrm((N_EVEN, 2, RG_BLOCKS, RG_BLOCK, RG_BLOCK), RG_BLOCK ** -0.5),
        "rg_bx": nrm((N_EVEN, 2, RG_WIDTH), 0.1),
        "rg_lambda": jnp.log(p0) - jnp.log1p(-p0),
        "na_w_qkv": nrm((N_ODD, D, 3 * D), D ** -0.5),
        "na_w_o": nrm((N_ODD, D, D), D ** -0.5),
        "na_q_g": gain((N_ODD, NA_HEAD_DIM)),
        "na_k_g": gain((N_ODD, NA_HEAD_DIM)),
        "na_rpb": nrm((N_ODD, NA_HEADS, 2 * NA_KR - 1, 2 * NA_KC - 1), 0.5),
        "ffn_w1": nrm((N_EVEN, D, D_FF), D ** -0.5),
        "ffn_w3": nrm((N_EVEN, D, D_FF), D ** -0.5),
        "ffn_w2": nrm((N_EVEN, D_FF, D), D_FF ** -0.5),
        "moe_router": nrm((N_ODD, D, N_EXPERTS), D ** -0.5),
        "moe_w1": nrm((N_ODD, N_EXPERTS, D, D_FF_EXPERT), D ** -0.5),
        "moe_w3": nrm((N_ODD, N_EXPERTS, D, D_FF_EXPERT), D ** -0.5),
        "moe_w2": nrm((N_ODD, N_EXPERTS, D_FF_EXPERT, D), D_FF_EXPERT ** -0.5),
    }


def reference(x, c, ctx, c_ctx, w_mod, b_mod, norm_mix_g, norm_ffn_g,
              ev_w_in, ev_w_out, hg_lb_logits, hg_onorm_g, rg_conv_w, rg_conv_b,
              rg_wa, rg_ba, rg_wx, rg_bx, rg_lambda,
              na_w_qkv, na_w_o, na_q_g, na_k_g, na_rpb,
              ffn_w1, ffn_w3, ffn_w2, moe_router, moe_w1, moe_w3, moe_w2):
    n_ctx = ctx.shape[1]
    lb_p = jax.nn.softmax(hg_lb_logits.astype(jnp.float32), axis=0)
    lb_all = jnp.cumsum(lb_p, axis=0) - lb_p[0]
    h, g = x, ctx
    for layer in range(DEPTH):
        j = layer // 2
        last = layer == DEPTH - 1
        sh1, sc1, gt1, sh2, sc2, gt2 = [t[:, None, :] for t in _adaln(c, w_mod[layer], b_mod[layer])]
        csh1, csc1, cgt1, csh2, csc2, cgt2 = _adaln(c_ctx, w_mod[layer], b_mod[layer])
        u_lat = _modulate(h, norm_mix_g[layer], sh1, sc1)
        u_ctx = _modulate(g, norm_mix_g[layer], csh1, csc1)
        if layer % 2 == 0:
            o_lat, o_ctx = _even_mixer(u_ctx, u_lat, ev_w_in[j], ev_w_out[j], lb_all[j], hg_onorm_g[j],
                                       rg_conv_w[j], rg_conv_b[j], rg_wa[j], rg_ba[j],
                                       rg_wx[j], rg_bx[j], rg_lambda[j], not last)
        else:
            o_lat, o_ctx = _na_mixer(u_ctx, u_lat, na_w_qkv[j], na_w_o[j], na_q_g[j], na_k_g[j],
                                     na_rpb[j], not last)
        h = h + gt1 * o_lat
        u_lat = _modulate(h, norm_ffn_g[layer], sh2, sc2)
        if last:
            tokens = u_lat
        else:
            g = g + cgt1 * o_ctx
            tokens = jnp.concatenate([_modulate(g, norm_ffn_g[layer], csh2, csc2), u_lat], axis=1)
        if layer % 2 == 0:
            y = _swiglu(tokens, ffn_w1[j], ffn_w3[j], ffn_w2[j])
        else:
            y = _moe(tokens, moe_router[j], moe_w1[j], moe_w3[j], moe_w2[j])
        if last:
            h = h + gt2 * y
        else:
            g = g + cgt2 * y[:, :n_ctx]
            h = h + gt2 * y[:, n_ctx:]
    return h
```

```python
import numpy as np
from contextlib import ExitStack
import concourse.bass as bass
import concourse.mybir as mybir
from concourse.bass_utils import run_bass_kernel_spmd

F32 = mybir.dt.float32
BF16 = mybir.dt.bfloat16
AF = mybir.ActivationFunctionType
ALU = mybir.AluOpType
AX = mybir.AxisListType

ENGS = ("pe", "act", "dve", "pool", "sp")


class Op:
    __slots__ = ("eng", "fn", "deps", "idx", "signal", "is_dma", "semkey", "seq")

    def __init__(self, eng, fn, is_dma=False, semkey=None):
        self.eng = eng
        self.fn = fn
        self.deps = []
        self.signal = False
        self.is_dma = is_dma
        self.semkey = semkey
        self.seq = None


class Prog:
    def __init__(self, nc):
        self.nc = nc
        self.ops = {e: [] for e in ENGS}
        self.last_writer = {}
        self.readers = {}
        self._yield = None

    def op(self, eng, fn, reads=(), writes=(), dma=False, semkey=None):
        o = Op(eng, fn, is_dma=dma, semkey=semkey)
        deps = set()
        for r in reads:
            w = self.last_writer.get(r)
            if w is not None:
                deps.add(w)
        for w_ in writes:
            w = self.last_writer.get(w_)
            if w is not None:
                deps.add(w)
            for rd in self.readers.get(w_, ()):
                deps.add(rd)
        o.deps = list(deps)
        for r in reads:
            self.readers.setdefault(r, []).append(o)
        for w_ in writes:
            self.last_writer[w_] = o
            self.readers[w_] = []
        o.idx = len(self.ops[eng])
        self.ops[eng].append(o)
        if self._yield is not None:
            self._yield()
        return o

    def interleave(self, fns):
        import threading
        n = len(fns)
        batons = [threading.Semaphore(0) for _ in range(n)]
        done = [False] * n
        errs = []
        main = threading.Semaphore(0)
        tl = threading.local()

        def pass_baton(i):
            for k in range(1, n + 1):
                jn = (i + k) % n
                if not done[jn]:
                    batons[jn].release()
                    return
            main.release()

        def yielder():
            i = getattr(tl, "idx", None)
            if i is None:
                return
            pass_baton(i)
            batons[i].acquire()

        def runner(i):
            batons[i].acquire()
            tl.idx = i
            try:
                fns[i]()
            except BaseException as e:
                errs.append(e)
            done[i] = True
            pass_baton(i)

        ths = [threading.Thread(target=runner, args=(i,)) for i in range(n)]
        self._yield = yielder
        for t in ths:
            t.start()
        batons[0].release()
        main.acquire()
        for t in ths:
            t.join()
        self._yield = None
        if errs:
            raise errs[0]

    def barrier(self):
        lasts = [self.ops[e][-1] for e in ENGS if self.ops[e]]
        for e in ENGS:
            o = Op(e, lambda eng: eng.nop())
            o.deps = list(lasts)
            o.idx = len(self.ops[e])
            self.ops[e].append(o)

    def mm(self, out, lhsT, rhs, start, stop, reads, writes):
        return self.op("pe", lambda e: e.matmul(out, lhsT, rhs, start=start, stop=stop), reads, writes)

    def tr(self, out, in_, ident, reads, writes):
        return self.op("pe", lambda e: e.transpose(out, in_, ident), reads, writes)

    def dma(self, out, in_, reads, writes, eng="sp", semkey=None):
        return self.op(eng, lambda e: e.dma_start(out=out, in_=in_), reads, writes, dma=True, semkey=semkey)

    def act(self, out, in_, func, reads, writes, scale=None, bias=None):
        kw = {}
        if scale is not None:
            kw["scale"] = scale
        if bias is not None:
            kw["bias"] = bias
        return self.op("act", lambda e: e.activation(out=out, in_=in_, func=func, **kw), reads, writes)

    def tt(self, out, in0, in1, op, reads, writes, eng="dve"):
        return self.op(eng, lambda e: e.tensor_tensor(out=out, in0=in0, in1=in1, op=op), reads, writes)

    def ts(self, out, in0, s1, s2, op0, op1, reads, writes, eng="dve"):
        if op1 is None:
            return self.op(eng, lambda e: e.tensor_scalar(out=out, in0=in0, scalar1=s1, scalar2=None, op0=op0), reads, writes)
        return self.op(eng, lambda e: e.tensor_scalar(out=out, in0=in0, scalar1=s1, scalar2=s2, op0=op0, op1=op1), reads, writes)

    def stt(self, out, in0, scalar, in1, op0, op1, reads, writes):
        return self.op("dve", lambda e: e.scalar_tensor_tensor(out=out, in0=in0, scalar=scalar, in1=in1, op0=op0, op1=op1), reads, writes)

    def cp(self, out, in_, reads, writes, eng="dve"):
        if eng == "act":
            return self.op("act", lambda e: e.activation(out=out, in_=in_, func=AF.Copy), reads, writes)
        return self.op(eng, lambda e: e.tensor_copy(out=out, in_=in_), reads, writes)

    def recip(self, out, in_, reads, writes):
        return self.op("dve", lambda e: e.reciprocal(out=out, in_=in_), reads, writes)

    def scan(self, out, d0, d1, init, reads, writes):
        return self.op("dve", lambda e: e.tensor_tensor_scan(out=out, data0=d0, data1=d1, initial=init, op0=ALU.mult, op1=ALU.add), reads, writes)

    def memset(self, ap, val, reads, writes, eng="dve"):
        return self.op(eng, lambda e: e.memset(ap, val), reads, writes)

    def emit(self, block, stack):
        nc = self.nc
        for e in ENGS:
            for o in self.ops[e]:
                for d in o.deps:
                    if d.is_dma or d.eng != o.eng or o.eng != "pe":
                        d.signal = True
        sems = {}
        for e in ("pe", "act", "dve", "pool", "sp"):
            sems[e] = stack.enter_context(nc.semaphore("s_" + e))
        dma_counts = {}
        for e in ENGS:
            for o in self.ops[e]:
                if o.is_dma and o.semkey not in sems:
                    sems[o.semkey] = stack.enter_context(nc.semaphore("d_" + str(o.semkey)))
                    dma_counts[o.semkey] = 0
        self.n_sems = len(sems)
        for e in ENGS:
            cnt = 0
            for o in self.ops[e]:
                if o.is_dma:
                    dma_counts[o.semkey] += 16
                    o.seq = dma_counts[o.semkey]
                elif o.signal:
                    cnt += 1
                    o.seq = cnt

        def run_engine(ename, eng):
            water = {}
            for o in self.ops[ename]:
                need = {}
                for d in o.deps:
                    if d.is_dma:
                        key = d.semkey
                    else:
                        if d.eng == ename and ename == "pe":
                            continue
                        key = d.eng
                    if d.seq is None:
                        continue
                    if need.get(key, 0) < d.seq:
                        need[key] = d.seq
                for key, v in need.items():
                    if water.get(key, 0) < v:
                        eng.wait_ge(sems[key], v)
                        water[key] = v
                ins = o.fn(eng)
                if o.is_dma:
                    ins.then_inc(sems[o.semkey], 16)
                elif o.signal:
                    ins.then_inc(sems[ename], 1)

        block.tensor(lambda eng: run_engine("pe", eng))
        block.scalar(lambda eng: run_engine("act", eng))
        block.vector(lambda eng: run_engine("dve", eng))
        block.gpsimd(lambda eng: run_engine("pool", eng))
        block.sync(lambda eng: run_engine("sp", eng))


DM = 1024
NCTX = 256
SEQ = 2048
T = NCTX + SEQ
DEPTH = 4
TILES = [(0, 256), (256, 512), (768, 512), (1280, 512), (1792, 512)]
DFF = 2816
NFC = DFF // 128
NEXP = 8
EPS = 1e-6
EVEN_IN = 3584
GRID_W = 64
ROWS = 32
NEG = -30000.0
ARENA_N = 28672
FG = 2
NSLOT = 2


def _chunks(v):
    v = np.asarray(v, np.float32)
    return np.ascontiguousarray(v.reshape(-1, 128).T)


class _Packer:
    def __init__(self):
        self.cols = []
        self.off = {}
        self.n = 0

    def add(self, name, arr):
        arr = np.asarray(arr, np.float32).reshape(128, -1)
        self.off[name] = (self.n, arr.shape[1])
        self.cols.append(arr)
        self.n += arr.shape[1]

    def pack(self):
        return np.ascontiguousarray(np.concatenate(self.cols, axis=1))


def _vec_layout(inp, b):
    pk = _Packer()
    cc = np.stack([_chunks(inp["c"][b]), _chunks(inp["c_ctx"])], axis=-1)
    pk.add("cc", cc)
    for l in range(DEPTH):
        pk.add("bmod%d" % l, _chunks(inp["b_mod"][l]))
        pk.add("gmix%d" % l, _chunks(inp["norm_mix_g"][l]))
        pk.add("gffn%d" % l, _chunks(inp["norm_ffn_g"][l]))
    lbl = np.stack([np.stack([_chunks(inp["hg_lb_logits"][jj, d]) for d in range(2)], 1) for jj in range(2)], 1)
    pk.add("lbl", lbl)
    for j in range(2):
        pk.add("onorm%d" % j, _chunks(inp["hg_onorm_g"][j]))
        pk.add("convw%d" % j, np.stack([_chunks(inp["rg_conv_w"][j, k]) for k in range(4)], 1))
        pk.add("convb%d" % j, _chunks(inp["rg_conv_b"][j]))
        pk.add("ba%d" % j, np.stack([_chunks(inp["rg_ba"][j, d]) for d in range(2)], 1))
        pk.add("bx%d" % j, np.stack([_chunks(inp["rg_bx"][j, d]) for d in range(2)], 1))
        pk.add("lam%d" % j, np.stack([_chunks(inp["rg_lambda"][j, d]) for d in range(2)], 1))
        pk.add("qg%d" % j, np.tile(np.asarray(inp["na_q_g"][j], np.float32), 2).reshape(128, 1))
        pk.add("kg%d" % j, np.tile(np.asarray(inp["na_k_g"][j], np.float32), 2).reshape(128, 1))
        wr = np.asarray(inp["moe_router"][j], np.float32).reshape(8, 128, 8).transpose(1, 0, 2)
        pk.add("wr%d" % j, wr)
    return pk.off, pk.pack()


def _consts():
    p = np.arange(128)
    ident = (p[:, None] == p[None, :]).astype(np.float32)
    ones = np.ones((128, 128), np.float32)
    blk = ((p[:, None] // 64) == (p[None, :] // 64)).astype(np.float32)
    s = (p % 64)[:, None]
    t = np.arange(64)[None, :]
    mF = -(s <= t).astype(np.float32)
    mB = -(s >= t).astype(np.float32)
    cm = np.broadcast_to((np.arange(512) % 64 != 0).astype(np.float32)[None, :], (128, 512))
    bf_part = np.concatenate([ident, ones, blk, mF, mB, cm], axis=1)
    return np.ascontiguousarray(np.concatenate([ident, bf_part], axis=1).astype(np.float32))


def _na_rel_tables():
    pats = {}
    rel = {}
    plist = []
    for m in range(16):
        for j in range(16):
            q = np.arange(128)
            qr = 2 * m + q // 64
            qc = q % 64
            kr = 2 * j + q // 64
            kc = q % 64
            rs = np.clip(qr - 4, 0, ROWS - 8)
            rowv = (kr[None, :] >= rs[:, None]) & (kr[None, :] < rs[:, None] + 8)
            ws = np.clip(qc - 8, 0, GRID_W - 16)
            colv = (kc[None, :] >= ws[:, None]) & (kc[None, :] < ws[:, None] + 16)
            valid = rowv & colv
            if not valid.any():
                continue
            dr = np.clip(kr[None, :] - qr[:, None] + 7, 0, 14)
            dc = np.clip(kc[None, :] - qc[:, None] + 15, 0, 30)
            key = (valid.tobytes(), (dr * valid).tobytes(), (dc * valid).tobytes())
            if key not in pats:
                pats[key] = len(plist)
                plist.append((valid, dr, dc))
            rel[(m, j)] = pats[key]
    return rel, plist


_NA_REL, _NA_PATS = _na_rel_tables()
NREL = len(_NA_PATS)


def _na_strip_tables():
    js_of = {m: [jj for jj in range(16) if (m, jj) in _NA_REL] for m in range(16)}
    strips = {}
    strip_of = {}
    blocks = []
    for m in range(16):
        key = tuple(_NA_REL[(m, jj)] for jj in js_of[m])
        if key not in strips:
            strips[key] = (len(blocks), len(key))
            blocks.extend(key)
        strip_of[m] = strips[key]
    return js_of, strip_of, blocks


_NA_JS, _NA_STRIP, _NA_STRIP_RELS = _na_strip_tables()
NSTRIP_BLK = len(_NA_STRIP_RELS)


def _na_bias_strips(rpb):
    blk = _na_bias_blocks(rpb)
    return np.ascontiguousarray(blk[:, _NA_STRIP_RELS].transpose(0, 1, 3, 2))


def _na_bias_blocks(rpb):
    rpb = np.asarray(rpb, np.float32)
    out = np.empty((16, NREL, 128, 128), np.float32)
    for r, (valid, dr, dc) in enumerate(_NA_PATS):
        g = rpb[:, dr, dc]
        out[:, r] = np.where(valid[None], g, np.float32(NEG))
    return out


DEBUG_STOP = None
SEQ_E1 = False
DBG_A = True
DBG_B = True
DBG_C = 'dve'


def build(layers, voff, mixer=True, ffn=True):
    nc = bass.Bass("TRN2", target_bir_lowering=False)
    dram = {}

    def DR(name, shape, kind="ExternalInput"):
        if name not in dram:
            dram[name] = nc.dram_tensor(name, list(shape), F32, kind=kind).ap()
        return dram[name]

    xin = DR("xin", [DM, T])
    hout = DR("hout", [DM, T], kind="ExternalOutput")
    vecs_d = DR("vecs", [128, voff["_n"]])
    consts_d = DR("consts", [128, 1152])

    st = ExitStack()
    with st:
        def sb(name, shape, dt):
            return st.enter_context(nc.sbuf_tensor("sb_" + name, shape, dt))

        h = sb("h", [128, 8, T], F32)
        u = sb("u", [128, 8, T], BF16)
        M = sb("M", [128, 8, T], BF16)
        arena = sb("arena", [128, ARENA_N], BF16)
        vecs = sb("vecs", [128, voff["_n"]], F32)
        identf = sb("identf", [128, 128], F32)
        maskf = sb("maskf", [128, 128], F32)
        cbf = sb("cbf", [128, 1024], BF16)
        modv = sb("modv", [128, 48, 2], F32)
        mvec = sb("mvec", [128, 6, 8, 2], F32)
        scc = sb("scc", [128, 8, 2], BF16)
        misc = sb("misc", [128, 64], F32)
        pbs = [st.enter_context(nc.psum_tensor("pb%d" % i, [128, 512], F32)) for i in range(8)]

        ident_bf = cbf[:, 0:128]
        ones_bf = cbf[:, 128:256]
        blk_bf = cbf[:, 256:384]
        maskF = cbf[:, 384:448]
        maskB = cbf[:, 448:512]
        cmask = cbf[:, 512:1024]

        P = Prog(nc)

        def V(name, *idx):
            o, n = voff[name]
            return vecs[:, o:o + n]

        class Arena:
            def __init__(self):
                self.off = 0

            def bf(self, n, shape=None):
                n2 = (n + 15) // 16 * 16
                assert self.off + n2 <= ARENA_N, ("arena overflow", self.off, n2)
                ap = arena[:, self.off:self.off + n]
                self.off += n2
                return ap

            def f32(self, n):
                n2 = (2 * n + 15) // 16 * 16
                assert self.off + n2 <= ARENA_N, ("arena overflow", self.off, n2)
                ap = arena[:, self.off:self.off + 2 * n].bitcast(F32)
                self.off += n2
                return ap

        P.dma(vecs[:], vecs_d, [], ["vecs"], semkey="vecs")
        P.dma(identf[:], consts_d[:, 0:128], [], ["identf"], semkey="consts")
        P.dma(maskf[:], consts_d[:, 512:640], [], ["maskf"], semkey="maskf")
        P.dma(cbf[:], consts_d[:, 128:1152], [], ["cbf"], eng="pool", semkey="cbf")
        xin_v = xin.rearrange("(c p) t -> p c t", p=128)
        for c in range(8):
            P.dma(h[:, c, :], xin_v[:, c, :], [], [("h", c, ti) for ti in range(5)], semkey="hload")
        o_cc = voff["cc"][0]
        ccv = vecs[:, o_cc:o_cc + 16]
        P.act(scc[:].rearrange("p k two -> p (k two)"), ccv, AF.Silu, ["vecs"], ["scc"])

        def adaln(l):
            P.barrier()
            ar = Arena()
            slots = [ar.bf(8 * 1024).rearrange("p (k f) -> p k f", k=8) for _ in range(2)]
            wm = DR("w_mod", [DEPTH, DM, 6 * DM])
            mp = pbs[0][:, 0:96].rearrange("p (j two) -> p j two", two=2)
            for gi in range(6):
                sl = slots[gi % 2]
                P.dma(sl, wm[l][:, gi * 1024:(gi + 1) * 1024].rearrange("(k p) f -> p k f", p=128),
                      [], [("admw", gi % 2)], eng="pool", semkey="admw%d" % (gi % 2))
                for j in range(8):
                    jj = gi * 8 + j
                    for k in range(8):
                        P.mm(mp[:, jj, :], sl[:, k, j * 128:(j + 1) * 128], scc[:, k, :], k == 0, k == 7,
                             [("admw", gi % 2), "scc"], [("admp", jj)])
            ob = voff["bmod%d" % l][0]
            bm = vecs[:, ob:ob + 48]
            P.tt(modv[:], mp, bm.unsqueeze(2).to_broadcast([128, 48, 2]), ALU.add,
                 [("admp", jj) for jj in range(48)] + ["vecs"], ["modv"])
            for which, (gname, isc, ish, igt) in enumerate((("gmix%d" % l, 1, 0, 2), ("gffn%d" % l, 4, 3, 5))):
                og = voff[gname][0]
                gv = vecs[:, og:og + 8]
                Aq = mvec[:, which * 3 + 0]
                P.ts(Aq, modv[:, isc * 8:(isc + 1) * 8, :], 1.0, None, ALU.add, None, ["modv"], ["mvec"])
                P.tt(Aq, Aq, gv.unsqueeze(2).to_broadcast([128, 8, 2]), ALU.mult, ["mvec", "vecs"], ["mvec"])
                P.cp(mvec[:, which * 3 + 1], modv[:, ish * 8:(ish + 1) * 8, :], ["modv", "mvec"], ["mvec"])
                P.cp(mvec[:, which * 3 + 2], modv[:, igt * 8:(igt + 1) * 8, :], ["modv", "mvec"], ["mvec"])

        def modulate(l, which, tiles, router_j=None, rl_ps=None):
            P.barrier()
            ar = Arena()
            sq = ar.bf(8 * 512).rearrange("p (k n) -> p k n", k=8)
            rt = ar.f32(512)
            rstd = ar.f32(512)
            tmp = [ar.f32(512) for _ in range(2)]
            uf = [ar.f32(512) for _ in range(2)]
            Aq, Bq = mvec[:, which * 3 + 0], mvec[:, which * 3 + 1]
            n = 0
            for ti in tiles:
                t0, N = TILES[ti]
                col = 1 if ti == 0 else 0
                for c in range(8):
                    P.act(sq[:, c, 0:N], h[:, c, t0:t0 + N], AF.Square, [("h", c, ti)], [("sq", c)])
                for c in range(8):
                    P.mm(pbs[0][:, 0:N], ones_bf, sq[:, c, 0:N], c == 0, c == 7, [("sq", c), "cbf"], ["ssps"])
                P.act(rt[:, 0:N], pbs[0][:, 0:N], AF.Ln, ["ssps"], ["rt"], scale=1.0 / DM, bias=EPS)
                P.act(rstd[:, 0:N], rt[:, 0:N], AF.Exp, ["rt"], ["rstd"], scale=-0.5)
                for c in range(8):
                    tb = tmp[n % 2]
                    P.stt(tb[:, 0:N], h[:, c, t0:t0 + N], Aq[:, c, col:col + 1], rstd[:, 0:N], ALU.mult, ALU.mult,
                          [("h", c, ti), "rstd", "mvec"], [("tmp", n % 2)])
                    if router_j is None:
                        P.act(u[:, c, t0:t0 + N], tb[:, 0:N], AF.Identity, [("tmp", n % 2), "mvec"], [("u", c, ti)],
                              bias=Bq[:, c, col:col + 1])
                    else:
                        ub = uf[n % 2]
                        P.act(ub[:, 0:N], tb[:, 0:N], AF.Identity, [("tmp", n % 2), "mvec"], [("uf", n % 2)],
                              bias=Bq[:, c, col:col + 1])
                        P.cp(u[:, c, t0:t0 + N], ub[:, 0:N], [("uf", n % 2)], [("u", c, ti)], eng="pool")
                        ow = voff["wr%d" % router_j][0]
                        P.mm(rl_ps[ti][0:8, 0:N], vecs[:, ow + c * 8:ow + (c + 1) * 8], ub[:, 0:N], c == 0, c == 7,
                             [("uf", n % 2), "vecs"], [("rlps", ti)])
                    n += 1

        def ffn_phase(l, tiles, experts, gatesT=None):
            ar = Arena()
            FGM = 4
            slots = []
            for s in range(2):
                w1s = ar.bf(8 * FGM * 128).rearrange("p (k f) -> p k f", k=8)
                w3s = ar.bf(8 * FGM * 128).rearrange("p (k f) -> p k f", k=8)
                w2s = ar.bf(FGM * 1024).rearrange("p (g d) -> p g d", g=FGM)
                slots.append((w1s, w3s, w2s))
            mo = [4608]

            def mbf(n):
                ap = Mflat[:, mo[0]:mo[0] + n]
                mo[0] += n
                assert mo[0] <= 8 * T
                return ap

            def mf32(n):
                return mbf(2 * n).bitcast(F32)

            gbc = mf32(T)
            actb = [mbf(FGM * 512).rearrange("p (g n) -> p g n", g=FGM) for _ in range(2)]
            s1b = [mf32(512) for _ in range(2)]
            s1g = [mf32(512) for _ in range(2)]
            G2 = mvec[:, 5]
            jobs = []
            for (w1a, w3a, w2a, ei) in experts:
                f0 = 0
                while f0 < NFC:
                    fg = min(FGM, NFC - f0)
                    jobs.append((w1a, w3a, w2a, ei, f0, fg))
                    f0 += fg

            def issue(i):
                w1a, w3a, w2a, ei, f0, fg = jobs[i]
                s = i % 2
                w1s, w3s, w2s = slots[s]
                fs = slice(f0 * 128, (f0 + fg) * 128)
                P.dma(w1s[:, :, 0:fg * 128], w1a[:, fs].rearrange("(k p) f -> p k f", p=128), [], [("fw1", s)], eng="pool", semkey="fw1_%d" % s)
                P.dma(w3s[:, :, 0:fg * 128], w3a[:, fs].rearrange("(k p) f -> p k f", p=128), [], [("fw3", s)], eng="pool", semkey="fw3_%d" % s)
                P.dma(w2s[:, 0:fg, :], w2a[fs, :].rearrange("(g p) d -> p g d", p=128), [], [("fw2", s)], eng="pool", semkey="fw2_%d" % s)

            nt = 0
            ng = 0
            ny = 0
            ybanks = [pbs[4], pbs[5], pbs[7]]
            issue(0)
            for i, (w1a, w3a, w2a, ei, f0, fg) in enumerate(jobs):
                if i + 1 < len(jobs):
                    issue(i + 1)
                s = i % 2
                w1s, w3s, w2s = slots[s]
                for ti in tiles:
                    t0, N = TILES[ti]
                    col = 1 if ti == 0 else 0
                    if ei is not None and f0 == 0:
                        P.mm(pbs[6][:, 0:N], identf[0:8, ei:ei + 1].to_broadcast([8, 128]), gatesT[0:8, t0:t0 + N],
                             True, True, ["gatesT", "identf"], ["gbps"])
                        P.cp(gbc[:, t0:t0 + N], pbs[6][:, 0:N], ["gbps"], [("gbc", ti)], eng="act")
                    ab = actb[nt % 2]
                    for g in range(fg):
                        hp1 = pbs[0 + (ng % 2)]
                        hp3 = pbs[2 + (ng % 2)]
                        for k in range(8):
                            P.mm(hp1[:, 0:N], w1s[:, k, g * 128:(g + 1) * 128], u[:, k, t0:t0 + N], k == 0, k == 7,
                                 [("fw1", s), ("u", k, ti)], [("hp1", ng % 2)])
                        for k in range(8):
                            P.mm(hp3[:, 0:N], w3s[:, k, g * 128:(g + 1) * 128], u[:, k, t0:t0 + N], k == 0, k == 7,
                                 [("fw3", s), ("u", k, ti)], [("hp3", ng % 2)])
                        sb1 = s1b[ng % 2]
                        P.act(sb1[:, 0:N], hp1[:, 0:N], AF.Silu, [("hp1", ng % 2)], [("s1", ng % 2)])
                        if ei is not None:
                            sg = s1g[ng % 2]
                            P.tt(sg[:, 0:N], hp3[:, 0:N], gbc[:, t0:t0 + N], ALU.mult, [("hp3", ng % 2), ("gbc", ti)], [("s1g", ng % 2)])
                            P.tt(ab[:, g, 0:N], sg[:, 0:N], sb1[:, 0:N], ALU.mult, [("s1g", ng % 2), ("s1", ng % 2)], [("act", nt % 2, g)])
                        else:
                            P.tt(ab[:, g, 0:N], sb1[:, 0:N], hp3[:, 0:N], ALU.mult, [("s1", ng % 2), ("hp3", ng % 2)],
                                 [("act", nt % 2, g)])
                        ng += 1
                    for dc in range(8):
                        yp = ybanks[ny % 3]
                        for g in range(fg):
                            P.mm(yp[:, 0:N], w2s[:, g, dc * 128:(dc + 1) * 128], ab[:, g, 0:N], g == 0, g == fg - 1,
                                 [("fw2", s), ("act", nt % 2, g)], [("yp", ny % 3)])
                        P.stt(h[:, dc, t0:t0 + N], yp[:, 0:N], G2[:, dc, col:col + 1], h[:, dc, t0:t0 + N],
                              ALU.mult, ALU.add, [("yp", ny % 3), ("h", dc, ti), "mvec"], [("h", dc, ti)])
                        ny += 1
                    nt += 1

        def dense_ffn(l, tiles):
            j = l // 2
            w1 = DR("ffn_w1", [2, DM, DFF])
            w3 = DR("ffn_w3", [2, DM, DFF])
            w2 = DR("ffn_w2", [2, DFF, DM])
            modulate(l, 1, tiles)
            P.barrier()
            ffn_phase(l, tiles, [(w1[j], w3[j], w2[j], None)])

        def moe_ffn(l, tiles):
            j = l // 2
            w1 = DR("moe_w1", [2, NEXP, DM, DFF])
            w3 = DR("moe_w3", [2, NEXP, DM, DFF])
            w2 = DR("moe_w2", [2, NEXP, DFF, DM])
            rl_ps = {ti: pbs[1 + ti] for ti in range(5)}
            modulate(l, 1, tiles, router_j=j, rl_ps=rl_ps)
            P.barrier()
            ar = Arena()
            lgT = ar.f32(T)
            ntile = 18
            L = ar.f32(ntile * 8).rearrange("p (a e) -> p a e", e=8)
            eq1 = ar.f32(ntile * 8).rearrange("p (a e) -> p a e", e=8)
            eq2 = ar.f32(ntile * 8).rearrange("p (a e) -> p a e", e=8)
            L2 = ar.f32(ntile * 8).rearrange("p (a e) -> p a e", e=8)
            gts = ar.f32(ntile * 8).rearrange("p (a e) -> p a e", e=8)
            m1 = ar.f32(ntile)
            m2 = ar.f32(ntile)
            dd = ar.f32(ntile)
            w1v = ar.f32(ntile)
            w2v = ar.f32(ntile)
            for ti in tiles:
                t0, N = TILES[ti]
                P.cp(lgT[0:8, t0:t0 + N], rl_ps[ti][0:8, 0:N], [("rlps", ti)], [("lgT", ti)], eng="act")
            a0 = 0 if 0 in tiles else 2
            LP = pbs[0][:, 0:ntile * 8].rearrange("p (a e) -> p a e", e=8)
            for a in range(a0, ntile):
                ti = 0 if a < 2 else 1 + (a - 2) // 4
                P.tr(LP[:, a, :], lgT[0:8, a * 128:(a + 1) * 128], identf[0:8, 0:8], [("lgT", ti), "identf"], [("LP", a)])
            rd = [("LP", a) for a in range(a0, ntile)]
            sl = slice(a0, ntile)
            na = ntile - a0
            P.cp(L[:, sl, :], LP[:, sl, :], rd, ["L"])
            P.op("dve", lambda e: e.tensor_reduce(out=m1[:, sl], in_=L[:, sl, :], axis=AX.X, op=ALU.max), ["L"], ["m1"])
            P.tt(eq1[:, sl, :], L[:, sl, :], m1[:, sl].unsqueeze(2).to_broadcast([128, na, 8]), ALU.is_equal, ["L", "m1"], ["eq1"])
            P.stt(L2[:, sl, :], eq1[:, sl, :], -1e30, L[:, sl, :], ALU.mult, ALU.add, ["eq1", "L"], ["L2"])
            P.op("dve", lambda e: e.tensor_reduce(out=m2[:, sl], in_=L2[:, sl, :], axis=AX.X, op=ALU.max), ["L2"], ["m2"])
            P.tt(eq2[:, sl, :], L2[:, sl, :], m2[:, sl].unsqueeze(2).to_broadcast([128, na, 8]), ALU.is_equal, ["L2", "m2"], ["eq2"])
            P.tt(dd[:, sl], m2[:, sl], m1[:, sl], ALU.subtract, ["m1", "m2"], ["dd"])
            P.act(dd[:, sl], dd[:, sl], AF.Exp, ["dd"], ["dd"])
            P.ts(w1v[:, sl], dd[:, sl], 1.0, None, ALU.add, None, ["dd"], ["w1v"])
            P.recip(w1v[:, sl], w1v[:, sl], ["w1v"], ["w1v"])
            P.tt(w2v[:, sl], dd[:, sl], w1v[:, sl], ALU.mult, ["dd", "w1v"], ["w2v"])
            P.tt(gts[:, sl, :], eq1[:, sl, :], w1v[:, sl].unsqueeze(2).to_broadcast([128, na, 8]), ALU.mult, ["eq1", "w1v"], ["gts"])
            P.tt(eq2[:, sl, :], eq2[:, sl, :], w2v[:, sl].unsqueeze(2).to_broadcast([128, na, 8]), ALU.mult, ["eq2", "w2v"], ["eq2"])
            P.tt(gts[:, sl, :], gts[:, sl, :], eq2[:, sl, :], ALU.add, ["gts", "eq2"], ["gts"])
            gatesT = misc_big["gatesT"]
            for a in range(a0, ntile):
                gp = pbs[1 + (a % 2)]
                P.tr(gp[0:8, 0:128], gts[:, a, :], identf[:, :], ["gts", "identf"], [("gp", a % 2)])
                P.cp(gatesT[0:8, a * 128:(a + 1) * 128], gp[0:8, 0:128], [("gp", a % 2)], ["gatesT"], eng="act")
            P.barrier()
            ffn_phase(l, tiles, [(w1[j, e], w3[j, e], w2[j, e], e) for e in range(NEXP)], gatesT=gatesT)

        Mflat = M[:, :, :].rearrange("p a t -> p (a t)")
        misc_big = {"gatesT": Mflat[:, 0:4608].bitcast(F32)}

        def out_proj(l, w_ap):
            ar = Arena()
            wo = ar.bf(8 * 1024).rearrange("p (k f) -> p k f", k=8)
            P.dma(wo, w_ap.rearrange("(k p) f -> p k f", p=128), [], ["wo"], eng="pool", semkey="wo")
            G1 = mvec[:, 2]
            n = 0
            for ti in range(5):
                t0, N = TILES[ti]
                col = 1 if ti == 0 else 0
                for dc in range(8):
                    yp = pbs[n % 2]
                    for k in range(8):
                        P.mm(yp[:, 0:N], wo[:, k, dc * 128:(dc + 1) * 128], M[:, k, t0:t0 + N], k == 0, k == 7,
                             ["wo", ("M", k, ti)], [("yp", n % 2)])
                    P.stt(h[:, dc, t0:t0 + N], yp[:, 0:N], G1[:, dc, col:col + 1], h[:, dc, t0:t0 + N], ALU.mult, ALU.add,
                          [("yp", n % 2), ("h", dc, ti), "mvec"], [("h", dc, ti)])
                    n += 1

        def even_mixer(l):
            j = l // 2
            w_in = DR("ev_w_in", [2, DM, EVEN_IN])[j]
            w_out = DR("ev_w_out", [2, DM, DM])[j]
            wa_d = DR("rg_wa", [2, 2, 8, 64, 64])
            wx_d = DR("rg_wx", [2, 2, 8, 64, 64])

            def wcols(dst, c0, n):
                return w_in[:, c0:c0 + n].rearrange("(k p) f -> p k f", p=128)

            P.barrier()
            ar = Arena()
            Vall = M[:, 4:8, :].rearrange("p a t -> p (a t)").rearrange("p (i f) -> p i f", f=512)
            wi = ar.bf(8 * 512).rearrange("p (k f) -> p k f", k=8)
            P.dma(wi, wcols(wi, 3 * 512, 512), [], ["wi"], eng="pool", semkey="wi")
            for a in range(18):
                ti = 0 if a < 2 else 1 + (a - 2) // 4
                vp = pbs[a % 2]
                for k in range(8):
                    P.mm(vp[:, :], u[:, k, a * 128:(a + 1) * 128], wi[:, k, :], k == 0, k == 7, [("u", k, ti), "wi"], [("vp", a % 2)])
                P.cp(Vall[:, a, :], vp[:, :], [("vp", a % 2)], [("V", a)], eng="act" if a % 2 else "dve")
            for hd in range(4):
                for ti in range(5):
                    t0, N = TILES[ti]
                    P.memset(M[:, hd, t0:t0 + N], 0.0, [], [("M", hd, ti)], eng=DBG_C)
            P.barrier()
            ar = Arena()
            lbv = ar.f32(8)
            lbv3 = lbv.rearrange("p (d c) -> p d c", d=2)
            if j == 0:
                P.memset(lbv, 0.0, [], ["lbv"])
            else:
                ol = voff["lbl"][0]
                l0 = vecs[:, ol:ol + 8]
                l1 = vecs[:, ol + 8:ol + 16]
                P.tt(lbv, l0, l1, ALU.subtract, ["vecs"], ["lbv"])
                P.act(lbv, lbv, AF.Exp, ["lbv"], ["lbv"])
                P.ts(lbv, lbv, 1.0, None, ALU.add, None, ["lbv"], ["lbv"])
                P.recip(lbv, lbv, ["lbv"], ["lbv"])

            def e1_chain(d):
                cn = "c%d" % d
                wz = ar.bf(8 * 128).rearrange("p (k f) -> p k f", k=8)
                wq = ar.bf(8 * 128).rearrange("p (k f) -> p k f", k=8)
                X = [ar.f32(512) for _ in range(5)]
                SQf = ar.bf(T)
                qt, kt, kh = ar.bf(512), ar.bf(512), ar.bf(512)
                KT = ar.bf(512).rearrange("p (a k) -> p a k", k=128)
                AT = ar.bf(256).rearrange("p (a s) -> p a s", s=64)
                AT32 = ar.f32(256).rearrange("p (a s) -> p a s", s=64)
                e1v, e12v = ar.f32(8), ar.f32(8)
                Sb = [ar.f32(128) for _ in range(2)]
                Tb = [ar.bf(128) for _ in range(2)]
                pz, pat, pop, pap = pbs[4 * d + 0], pbs[4 * d + 1], pbs[4 * d + 2], pbs[4 * d + 3]
                bA, bB, bC, bD = (cn, "bA"), (cn, "bB"), (cn, "bC"), (cn, "bD")
                trp = pz[:, 0:256].bitcast(BF16)
                P.memset(AT32, 0.0, [], [(cn, "AT32")], eng="pool")
                order = [0, 1, 2, 3, 4] if d == 0 else [0, 4, 3, 2, 1]
                r_idx = 31 if d == 0 else 32
                l_idx = 63 if d == 0 else 0
                msk = (maskf[:, 0:64] if d == 0 else maskf[:, 64:128]).bitcast(mybir.dt.uint32)
                for hd in range(4):
                    P.dma(wz, wcols(wz, 512 * (1 + d) + hd * 128, 128), [], [(cn, "wz")], eng="pool", semkey="wz%d" % d)
                    P.dma(wq, wcols(wq, hd * 128, 128), [], [(cn, "wq")], eng="pool", semkey="wq%d" % d)
                    for ti in range(5):
                        t0, N = TILES[ti]
                        for k in range(8):
                            P.mm(pz[:, 0:N], wq[:, k, :], u[:, k, t0:t0 + N], k == 0, k == 7, [(cn, "wq"), ("u", k, ti)], [bA])
                        P.act(SQf[:, t0:t0 + N], pz[:, 0:N], AF.Silu, [bA], [(cn, "SQ", ti)])
                    Sc = 0
                    P.memset(Sb[0], 0.0, [], [(cn, "S", 0)])
                    for ti in order:
                        t0, N = TILES[ti]
                        nch = N // 64
                        na = N // 128
                        for k in range(8):
                            P.mm(pz[:, 0:N], wz[:, k, :], u[:, k, t0:t0 + N], k == 0, k == 7, [(cn, "wz"), ("u", k, ti)], [bA])
                        E, L1, L2, Fv, DL = [x[:, 0:N] for x in X]
                        xn = [(cn, "X%d" % i) for i in range(5)]
                        P.act(E, pz[:, 0:N], AF.Exp, [bA], [xn[0]], scale=-1.0)
                        P.act(L1, E, AF.Ln, [xn[0]], [xn[1]], bias=1.0)
                        P.act(L2, E, AF.Ln, [xn[0], "lbv"], [xn[2]], bias=1.0, scale=lbv3[:, d, hd:hd + 1])
                        P.tt(L2, L2, L1, ALU.subtract, [xn[2], xn[1]], [xn[2]])
                        P.act(Fv, L2, AF.Exp, [xn[2]], [xn[3]])
                        Bv = E
                        if d == 0:
                            P.scan(Bv, cmask[:, 0:N], L2, 0.0, [xn[2], "cbf", xn[0]], [xn[0]])
                        else:
                            P.scan(Bv[:, ::-1], cmask[:, 0:N], L2[:, ::-1], 0.0, [xn[2], "cbf", xn[0]], [xn[0]])
                        B3 = Bv.rearrange("p (a b) -> p a b", b=64)
                        Dv = L1
                        D3 = Dv.rearrange("p (a b) -> p a b", b=64)
                        P.tt(D3, B3, B3[:, :, r_idx:r_idx + 1].to_broadcast([128, nch, 64]), ALU.subtract, [xn[0], xn[1]], [xn[1]])
                        DL3 = DL.rearrange("p (a b) -> p a b", b=64)
                        P.tt(DL3, B3[:, :, l_idx:l_idx + 1].to_broadcast([128, nch, 64]), B3, ALU.subtract, [xn[0]], [xn[4]])
                        EK = L2
                        P.act(EK, Dv, AF.Exp, [xn[1], xn[2]], [xn[2]], scale=-1.0)
                        P.act(Dv, Dv, AF.Exp, [xn[1]], [xn[1]])
                        P.act(DL, DL, AF.Exp, [xn[4]], [xn[4]])
                        P.act(e1v[:, 0:nch], B3[:, :, r_idx], AF.Exp, [xn[0], (cn, "e")], [(cn, "e")])
                        P.act(e12v[:, 0:nch], B3[:, :, l_idx], AF.Exp, [xn[0], (cn, "e")], [(cn, "e")])
                        P.tt(qt[:, 0:N], SQf[:, t0:t0 + N], Dv, ALU.mult, [(cn, "SQ", ti), xn[1], (cn, "qt")], [(cn, "qt")])
                        P.stt(kt[:, 0:N], Fv, -1.0, EK, ALU.add, ALU.mult, [xn[3], xn[2], (cn, "kt")], [(cn, "kt")])
                        P.stt(kh[:, 0:N], Fv, -1.0, DL, ALU.add, ALU.mult, [xn[3], xn[4], (cn, "kh")], [(cn, "kh")])
                        for a in range(na):
                            P.tr(trp[:, a * 128:(a + 1) * 128], kh[:, a * 128:(a + 1) * 128], ident_bf, [(cn, "kh"), "cbf"], [bA])
                        P.cp(KT[:, 0:na, :].rearrange("p a k -> p (a k)"), trp[:, 0:N], [bA, (cn, "KT")], [(cn, "KT")], eng="dve")
                        atp = pat[:, 0:na * 64].rearrange("p (a s) -> p a s", s=64)
                        for jc in range(nch):
                            a, hf = jc // 2, jc % 2
                            P.mm(atp[hf * 64:(hf + 1) * 64, a, :], kt[:, jc * 64:(jc + 1) * 64], qt[:, jc * 64:(jc + 1) * 64],
                                 True, True, [(cn, "kt"), (cn, "qt")], [bB])
                        for a in range(na):
                            P.op("dve", lambda e, o_=AT32[:, a, :], m_=msk, d_=atp[:, a, :]: e.copy_predicated(out=o_, mask=m_, data=d_),
                                 [bB, "maskf", (cn, "AT32")], [(cn, "AT32")])
                        P.cp(AT[:, 0:na, :], AT32[:, 0:na, :], [(cn, "AT32"), (cn, "AT")], [(cn, "AT")], eng="act")
                        corder = list(range(nch)) if d == 0 else list(range(nch - 1, -1, -1))

                        def ap_of(pos):
                            jc_ = corder[pos]
                            bank, nm = (pap, bD) if jc_ % 2 == 0 else (pat, bB)
                            return bank[:, (jc_ // 2) * 128:(jc_ // 2 + 1) * 128], nm

                        def issue_ap(pos):
                            jc = corder[pos]
                            a, hf = jc // 2, jc % 2
                            ga = (t0 // 128) + a
                            hs = slice(hf * 64, (hf + 1) * 64)
                            Ap, nm = ap_of(pos)
                            P.mm(Ap, KT[hs, a, :], Vall[hs, ga, hd * 128:(hd + 1) * 128], True, True, [(cn, "KT"), ("V", ga)], [nm])

                        for pos in range(nch):
                            issue_ap(pos)
                        for pos, jc in enumerate(corder):
                            a, hf = jc // 2, jc % 2
                            ga = (t0 // 128) + a
                            hs = slice(hf * 64, (hf + 1) * 64)
                            cs = slice(jc * 64, (jc + 1) * 64)
                            Vh = Vall[hs, ga, hd * 128:(hd + 1) * 128]
                            Ap, nm = ap_of(pos)
                            S_old, S_new = Sb[Sc % 2], Sb[(Sc + 1) % 2]
                            tb = Tb[Sc % 2]
                            P.act(tb, S_old, AF.Copy, [(cn, "S", Sc % 2), (cn, "e")], [(cn, "Tb", Sc % 2)], scale=e1v[:, jc:jc + 1])
                            P.mm(pop[:, cs], tb, qt[:, cs], True, False, [(cn, "Tb", Sc % 2), (cn, "qt")], [bC])
                            P.mm(pop[:, cs], Vh, AT[hs, a, :], False, True, [(cn, "AT"), ("V", ga)], [bC])
                            P.stt(S_new, S_old, e12v[:, jc:jc + 1], Ap, ALU.mult, ALU.add,
                                  [(cn, "S", Sc % 2), nm, (cn, "e")], [(cn, "S", (Sc + 1) % 2)])
                            Sc += 1
                        P.tt(M[:, hd, t0:t0 + N], M[:, hd, t0:t0 + N], pop[:, 0:N], ALU.subtract, [bC, ("M", hd, ti)], [("M", hd, ti)])

            if SEQ_E1:
                e1_chain(0)
                e1_chain(1)
            else:
                P.interleave([lambda: e1_chain(0), lambda: e1_chain(1)])

            if DEBUG_STOP == 'E1':
                return 'dumpM'
            P.barrier()
            ar = Arena()
            xp = ar.f32(T)
            Aa = ar.f32(T)
            BC = [ar.f32(T) for _ in range(2)]
            wxy = [ar.bf(8 * 128).rearrange("p (k f) -> p k f", k=8) for _ in range(2)]
            bd = [[ar.bf(128) for _ in range(2)] for _ in range(2)]
            xr = ar.f32(512)
            xrb = ar.bf(512)
            rr = ar.f32(512)
            ig = ar.f32(512)
            a2 = ar.f32(512)
            ysb = ar.f32(512)
            yt = ar.f32(512)
            sv = ar.f32(4)
            sv3 = sv.rearrange("p (d two) -> p d two", d=2)
            cw_o = voff["convw%d" % j][0]
            cb_o = voff["convb%d" % j][0]
            ba_o = voff["ba%d" % j][0]
            bx_o = voff["bx%d" % j][0]
            lam_o = voff["lam%d" % j][0]
            for c in range(4):
                P.barrier()
                P.dma(wxy[0], wcols(None, 2560 + c * 128, 128), [], ["wxy0"], eng="pool", semkey="wxy0")
                P.dma(wxy[1], wcols(None, 3072 + c * 128, 128), [], ["wxy1"], eng="pool", semkey="wxy1")
                for d in range(2):
                    for mi, wd in enumerate((wa_d, wx_d)):
                        P.memset(bd[d][mi], 0.0, [], [("bd", d, mi)], eng="pool")
                        for hb in range(2):
                            P.dma(bd[d][mi][hb * 64:(hb + 1) * 64, hb * 64:(hb + 1) * 64], wd[j, d, 2 * c + hb],
                                  [("bd", d, mi)], [("bd", d, mi)], eng="pool", semkey="bd%d%d" % (d, mi))
                    lamv = vecs[:, lam_o + d * 4 + c:lam_o + d * 4 + c + 1]
                    P.act(sv3[:, d, 0:1], lamv, AF.Exp, ["vecs"], ["sv"], scale=-1.0)
                    P.act(sv3[:, d, 0:1], sv3[:, d, 0:1], AF.Ln, ["sv"], ["sv"], bias=1.0)
                    P.ts(sv3[:, d, 1:2], sv3[:, d, 0:1], -16.0, None, ALU.mult, None, ["sv"], ["sv"])
                    P.ts(sv3[:, d, 0:1], sv3[:, d, 0:1], -8.0, None, ALU.mult, None, ["sv"], ["sv"])
                for ti in range(5):
                    t0, N = TILES[ti]
                    pp = pbs[ti % 2]
                    for k in range(8):
                        P.mm(pp[:, 0:N], wxy[0][:, k, :], u[:, k, t0:t0 + N], k == 0, k == 7, ["wxy0", ("u", k, ti)], [("pp", ti % 2)])
                    P.cp(xp[:, t0:t0 + N], pp[:, 0:N], [("pp", ti % 2)], ["xp"], eng="act")
                for d in range(2):
                    for ti in range(5):
                        t0, N = TILES[ti]
                        s0, s1 = (0, NCTX) if ti == 0 else (NCTX, T)
                        xv = xr[:, 0:N]
                        P.ts(xv, xp[:, t0:t0 + N], vecs[:, cw_o + 2 * 4 + c:cw_o + 2 * 4 + c + 1], vecs[:, cb_o + c:cb_o + c + 1],
                             ALU.mult, ALU.add, ["xp", "vecs"], ["xr"])
                        for tap, sh in ((0, -2), (1, -1), (3, 1)):
                            lo = max(t0, s0 - sh)
                            hi = min(t0 + N, s1 - sh)
                            wv = vecs[:, cw_o + tap * 4 + c:cw_o + tap * 4 + c + 1]
                            P.stt(xr[:, lo - t0:hi - t0], xp[:, lo + sh:hi + sh], wv, xr[:, lo - t0:hi - t0], ALU.mult, ALU.add,
                                  ["xp", "xr", "vecs"], ["xr"])
                        P.cp(xrb[:, 0:N], xv, ["xr"], ["xrb"], eng="pool")
                        rp, ip = pbs[2], pbs[3]
                        P.mm(rp[:, 0:N], bd[d][0], xrb[:, 0:N], True, True, [("bd", d, 0), "xrb"], ["rp"])
                        P.mm(ip[:, 0:N], bd[d][1], xrb[:, 0:N], True, True, [("bd", d, 1), "xrb"], ["ip"])
                        P.act(rr[:, 0:N], rp[:, 0:N], AF.Sigmoid, ["rp", "vecs"], ["rr"], bias=vecs[:, ba_o + d * 4 + c:ba_o + d * 4 + c + 1])
                        P.act(ig[:, 0:N], ip[:, 0:N], AF.Sigmoid, ["ip", "vecs"], ["ig"], bias=vecs[:, bx_o + d * 4 + c:bx_o + d * 4 + c + 1])
                        P.act(Aa[:, t0:t0 + N], rr[:, 0:N], AF.Exp, ["rr", "sv"], ["Aa"], scale=sv3[:, d, 0:1])
                        P.act(a2[:, 0:N], rr[:, 0:N], AF.Exp, ["rr", "sv"], ["a2"], scale=sv3[:, d, 1:2])
                        P.ts(a2[:, 0:N], a2[:, 0:N], -1.0, 1.0, ALU.mult, ALU.add, ["a2"], ["a2"])
                        P.ts(a2[:, 0:N], a2[:, 0:N], 1e-24, None, ALU.max, None, ["a2"], ["a2"])
                        P.act(a2[:, 0:N], a2[:, 0:N], AF.Sqrt, ["a2"], ["a2"])
                        P.tt(ig[:, 0:N], ig[:, 0:N], xv, ALU.mult, ["ig", "xr"], ["ig"])
                        P.tt(BC[d][:, t0:t0 + N], a2[:, 0:N], ig[:, 0:N], ALU.mult, ["a2", "ig"], [("BC", d)])
                    if d == 0:
                        P.scan(BC[0][:, :], Aa[:, :], BC[0][:, :], 0.0, ["Aa", ("BC", 0)], [("BC", 0)])
                    else:
                        P.scan(BC[1][:, NCTX - 1::-1], Aa[:, NCTX - 1::-1], BC[1][:, NCTX - 1::-1], 0.0, ["Aa", ("BC", 1)], [("BC", 1)])
                        P.scan(BC[1][:, T - 1:NCTX - 1:-1], Aa[:, T - 1:NCTX - 1:-1], BC[1][:, T - 1:NCTX - 1:-1], BC[1][:, 0:1],
                               ["Aa", ("BC", 1)], [("BC", 1)])
                for ti in range(5):
                    t0, N = TILES[ti]
                    pp = pbs[ti % 2]
                    for k in range(8):
                        P.mm(pp[:, 0:N], wxy[1][:, k, :], u[:, k, t0:t0 + N], k == 0, k == 7, ["wxy1", ("u", k, ti)], [("pp", ti % 2)])
                    yv, tv = ysb[:, 0:N], yt[:, 0:N]
                    P.cp(yv, pp[:, 0:N], [("pp", ti % 2)], ["ysb"], eng="act")
                    P.tt(tv, yv, yv, ALU.mult, ["ysb"], ["yt"])
                    P.ts(tv, tv, 0.044715, 1.0, ALU.mult, ALU.add, ["yt"], ["yt"])
                    P.tt(tv, tv, yv, ALU.mult, ["yt", "ysb"], ["yt"])
                    P.act(tv, tv, AF.Sigmoid, ["yt"], ["yt"], scale=1.5957691216057308)
                    P.tt(tv, tv, yv, ALU.mult, ["yt", "ysb"], ["yt"])
                    P.tt(yv, BC[0][:, t0:t0 + N], BC[1][:, t0:t0 + N], ALU.add, [("BC", 0), ("BC", 1), "ysb"], ["ysb"])
                    P.tt(M[:, 4 + c, t0:t0 + N], yv, tv, ALU.mult, ["ysb", "yt"], [("M", 4 + c, ti)])

            if DEBUG_STOP == 'E2':
                return 'dumpM'
            P.barrier()
            ar = Arena()
            wg = ar.bf(8 * 512).rearrange("p (k f) -> p k f", k=8)
            sq = ar.bf(4 * 512).rearrange("p (k n) -> p k n", k=4)
            rt = ar.f32(512)
            rstd = ar.f32(512)
            sg = [ar.f32(512) for _ in range(2)]
            t1 = [ar.f32(512) for _ in range(2)]
            on_o = voff["onorm%d" % j][0]
            P.dma(wg, wcols(None, 2048, 512), [], ["wg"], eng="pool", semkey="wg")
            n = 0
            for ti in range(5):
                t0, N = TILES[ti]
                for c in range(4):
                    P.act(sq[:, c, 0:N], M[:, c, t0:t0 + N], AF.Square, [("M", c, ti)], [("sq", c)])
                for c in range(4):
                    P.mm(pbs[0][:, 0:N], ones_bf, sq[:, c, 0:N], c == 0, c == 3, [("sq", c), "cbf"], ["ssps"])
                P.act(rt[:, 0:N], pbs[0][:, 0:N], AF.Ln, ["ssps"], ["rt"], scale=1.0 / 512, bias=EPS)
                P.act(rstd[:, 0:N], rt[:, 0:N], AF.Exp, ["rt"], ["rstd"], scale=-0.5)
                for c in range(4):
                    gp = pbs[1 + n % 2]
                    for k in range(8):
                        P.mm(gp[:, 0:N], wg[:, k, c * 128:(c + 1) * 128], u[:, k, t0:t0 + N], k == 0, k == 7, ["wg", ("u", k, ti)], [("gp", n % 2)])
                    P.act(sg[n % 2][:, 0:N], gp[:, 0:N], AF.Silu, [("gp", n % 2)], [("sg", n % 2)])
                    P.stt(t1[n % 2][:, 0:N], M[:, c, t0:t0 + N], vecs[:, on_o + c:on_o + c + 1], rstd[:, 0:N], ALU.mult, ALU.mult,
                          [("M", c, ti), "rstd", "vecs"], [("t1", n % 2)])
                    P.tt(M[:, c, t0:t0 + N], t1[n % 2][:, 0:N], sg[n % 2][:, 0:N], ALU.mult, [("t1", n % 2), ("sg", n % 2), ("sq", c)], [("M", c, ti)])
                    n += 1
            if DEBUG_STOP == 'E3':
                return 'dumpM'
            P.barrier()
            out_proj(l, w_out)

        def na_mixer(l, with_ctx):
            j = l // 2
            wqkv = DR("na_w_qkv", [2, DM, 3 * DM])[j]
            w_o = DR("na_w_o", [2, DM, DM])[j]
            nab = DR("nab", [2, 16, NSTRIP_BLK, 128, 128])[j]
            P.barrier()
            ar = Arena()
            Qc = ar.bf(T)
            Kc = ar.bf(T)
            VA = ar.bf(18 * 130).rearrange("p (a f) -> p a f", f=130)
            w3 = [ar.bf(8 * 128).rearrange("p (k f) -> p k f", k=8) for _ in range(3)]
            bias = ar.bf(2 * NSTRIP_BLK * 128).rearrange("p (r q) -> p r q", q=128)
            sqb = [ar.bf(512) for _ in range(2)]
            rt = [ar.f32(512) for _ in range(2)]
            rstd = [ar.f32(512) for _ in range(2)]
            PT = [[ar.bf(512) for _ in range(2)] for _ in range(2)]
            OTt = [[ar.bf(128) for _ in range(2)] for _ in range(2)]
            rec = [[ar.f32(2) for _ in range(2)] for _ in range(2)]
            gs = ar.f32(2)
            P.ts(gs[:, 0:1], V("qg%d" % j), 0.125, None, ALU.mult, None, ["vecs"], ["gs"])
            P.cp(gs[:, 1:2], V("kg%d" % j), ["vecs", "gs"], ["gs"])
            VA4 = VA.rearrange("p a (h e) -> p a h e", h=2)
            P.memset(VA4[:, :, :, 64:65], 1.0, [], ["VAones"], eng="pool")
            for h_ in range(2):
                for b_ in range(2):
                    P.memset(OTt[h_][b_], 0.0, [], [("ot", h_, b_)], eng="pool")
            trps = [pbs[0][:, 0:64].bitcast(BF16), pbs[1][:, 0:64].bitcast(BF16)]
            qtiles = list(range(5)) if with_ctx else [1, 2, 3, 4]
            qlist = ([0, 1] if with_ctx else []) + list(range(2, 18))
            for c in range(8):
                P.barrier()
                for i in range(3):
                    P.dma(w3[i], wqkv[:, i * DM + c * 128:i * DM + (c + 1) * 128].rearrange("(k p) f -> p k f", p=128),
                          [], [("w3", i)], eng="pool", semkey="w3%d" % i)
                P.dma(bias, nab[2 * c:2 * c + 2].rearrange("h r k q -> k (h r) q"), [], ["bias"], eng="pool", semkey="nabias")

                def qk_stream(which):
                    dst, tl = (Qc, qtiles) if which == 0 else (Kc, list(range(5)))
                    pp, ssb = pbs[2 * which], pbs[2 * which + 1]
                    for ti in tl:
                        t0, N = TILES[ti]
                        for k in range(8):
                            P.mm(pp[:, 0:N], w3[which][:, k, :], u[:, k, t0:t0 + N], k == 0, k == 7, [("w3", which), ("u", k, ti)], [("pp", which)])
                        P.act(sqb[which][:, 0:N], pp[:, 0:N], AF.Square, [("pp", which)], [("sqb", which)])
                        P.mm(ssb[:, 0:N], blk_bf, sqb[which][:, 0:N], True, True, [("sqb", which), "cbf"], [("ssps", which)])
                        P.act(rt[which][:, 0:N], ssb[:, 0:N], AF.Ln, [("ssps", which)], [("rt", which)], scale=1.0 / 64, bias=EPS)
                        P.act(rstd[which][:, 0:N], rt[which][:, 0:N], AF.Exp, [("rt", which)], [("rstd", which)], scale=-0.5)
                        P.stt(dst[:, t0:t0 + N], pp[:, 0:N], gs[:, which:which + 1], rstd[which][:, 0:N], ALU.mult, ALU.mult,
                              [("pp", which), ("rstd", which), "gs"], [("qk", which, ti)])

                def v_stream():
                    for a in range(18):
                        ti = 0 if a < 2 else 1 + (a - 2) // 4
                        vp = pbs[4 + a % 2]
                        for k in range(8):
                            P.mm(vp[:, 0:128], u[:, k, a * 128:(a + 1) * 128], w3[2][:, k, :], k == 0, k == 7, [("u", k, ti), ("w3", 2)], [("vp", a % 2)])
                        P.cp(VA4[:, a, :, 0:64], vp[:, 0:128].rearrange("p (h e) -> p h e", h=2), [("vp", a % 2), "VAones"], [("VA", a)],
                             eng="pool" if False else "dve")

                P.interleave([lambda: qk_stream(0), lambda: qk_stream(1), v_stream])
                P.barrier()

                def att_stream(hh):
                    hs = slice(hh * 64, (hh + 1) * 64)
                    spb = (pbs[4], pbs[5]) if hh == 0 else (pbs[2], pbs[3])
                    op_ = pbs[6 + hh][:, 0:65]
                    npt = 0
                    for qi, qa in enumerate(qlist):
                        tiq = 0 if qa < 2 else 1 + (qa - 2) // 4
                        if qa < 2:
                            kts = [(0, None), (1, None)]
                        else:
                            m = qa - 2
                            so, sn = _NA_STRIP[m]
                            js = _NA_JS[m]
                            kts = [(2 + jj, so + i_) for i_, jj in enumerate(js)] + [(0, None), (1, None)]
                        groups = [kts[i:i + 4] for i in range(0, len(kts), 4)]
                        first = True
                        for gi, grp in enumerate(groups):
                            sp_ = spb[npt % 2]
                            pt = PT[hh][npt % 2]
                            pk = ("pt", hh, npt % 2)
                            spn = ("sp", hh, npt % 2)
                            nloc = sum(1 for (_, rl) in grp if rl is not None)
                            if nloc:
                                b0 = grp[0][1]
                                P.mm(sp_[:, 0:nloc * 128], ident_bf, bias[:, hh * NSTRIP_BLK + b0:hh * NSTRIP_BLK + b0 + nloc, :].rearrange("p r q -> p (r q)"),
                                     True, False, ["bias", "cbf"], [spn])
                            for si, (ka, rl) in enumerate(grp):
                                tik = 0 if ka < 2 else 1 + (ka - 2) // 4
                                P.mm(sp_[:, si * 128:(si + 1) * 128], Kc[hs, ka * 128:(ka + 1) * 128], Qc[hs, qa * 128:(qa + 1) * 128],
                                     rl is None, (rl is None) or (si == nloc - 1), [("qk", 1, tik), ("qk", 0, tiq)], [spn])
                            ng_ = len(grp)
                            P.act(pt[:, 0:ng_ * 128], sp_[:, 0:ng_ * 128], AF.Exp, [spn], [pk])
                            for si, (ka, rl) in enumerate(grp):
                                last = (gi == len(groups) - 1) and (si == ng_ - 1)
                                P.mm(op_, pt[:, si * 128:(si + 1) * 128], VA[:, ka, hh * 65:(hh + 1) * 65], first, last,
                                     [("VA", ka), pk], [("op", hh)])
                                first = False
                            npt += 1
                        ot = OTt[hh][qi % 2]
                        rc = rec[hh][qi % 2]
                        o2 = pbs[6 + hh]
                        P.recip(rc[:, 0:1], o2[:, 64:65], [("op", hh), ("rec", hh, qi % 2)], [("rec", hh, qi % 2)])
                        P.ts(ot[:, hs], o2[:, 0:64], rc[:, 0:1], None, ALU.mult, None,
                             [("op", hh), ("rec", hh, qi % 2), ("ot", hh, qi % 2)], [("ot", hh, qi % 2)])
                        tp_ = trps[hh]
                        P.tr(tp_, ot, ident_bf, [("ot", hh, qi % 2), "cbf"], [("trp", hh)])
                        P.cp(M[hs, c, qa * 128:(qa + 1) * 128], tp_[hs, :], [("trp", hh)], [("Mq", c, qa, hh)], eng="act")

                P.interleave([lambda: att_stream(0), lambda: att_stream(1)])
                for ti in range(5):
                    P.op("dve", lambda e: e.nop(), [("Mq", c, qa, h_) for qa in qlist for h_ in range(2)], [("M", c, ti)])
            if not with_ctx:
                for c in range(8):
                    P.memset(M[:, c, 0:NCTX], 0.0, [("M", c, 0)], [("M", c, 0)], eng="pool")

            if DEBUG_STOP == 'NA':
                return 'dumpM'
            P.barrier()
            out_proj(l, w_o)

        dump_m = False
        for l in layers:
            last = (l == DEPTH - 1)
            adaln(l)
            if mixer:
                modulate(l, 0, list(range(5)))
                if l % 2 == 0:
                    rv = even_mixer(l)
                else:
                    rv = na_mixer(l, not last)
                if rv == 'dumpM':
                    dump_m = True
                    break
            if ffn:
                tiles = [1, 2, 3, 4] if last else list(range(5))
                if l % 2 == 0:
                    dense_ffn(l, tiles)
                else:
                    moe_ffn(l, tiles)
        P.barrier()
        hout_v = hout.rearrange("(c p) t -> p c t", p=128)
        for c in range(8):
            if dump_m:
                P.dma(hout_v[:, c, :], M[:, c, :], [], [("hout", c)], eng="pool", semkey="hout")
            else:
                P.dma(hout_v[:, c, :], h[:, c, :], [("h", c, ti) for ti in range(5)], [("hout", c)], semkey="hout")
        P.op("sp", lambda e: e.nop(), [("hout", c) for c in range(8)], [])
        with nc.Block() as block:
            P.emit(block, st)
        build.last_stats = dict(n_ops={e: len(P.ops[e]) for e in ENGS}, n_sems=P.n_sems)
    return nc


WEIGHT_KEYS = ["w_mod", "ev_w_in", "ev_w_out", "rg_wa", "rg_wx", "na_w_qkv", "na_w_o",
               "ffn_w1", "ffn_w3", "ffn_w2", "moe_w1", "moe_w3", "moe_w2"]


def _needed_keys(layers, mixer=True, ffn=True):
    keys = ["w_mod"]
    for l in layers:
        if l % 2 == 0:
            if mixer:
                keys += ["ev_w_in", "ev_w_out", "rg_wa", "rg_wx"]
            if ffn:
                keys += ["ffn_w1", "ffn_w3", "ffn_w2"]
        else:
            if mixer:
                keys += ["na_w_qkv", "na_w_o", "nab"]
            if ffn:
                keys += ["moe_w1", "moe_w3", "moe_w2"]
    return sorted(set(keys))


def run_layers(inp, state, layers, cores=8, mixer=True, ffn=True, trace=False):
    offs = None
    vec_arrs = []
    for b in range(cores):
        off, arr = _vec_layout(inp, b)
        offs = off
        vec_arrs.append(arr)
    offs = dict(offs)
    offs["_n"] = vec_arrs[0].shape[1]
    nc = build(layers, offs, mixer=mixer, ffn=ffn)
    consts = _consts()
    keys = _needed_keys(layers, mixer, ffn)
    shared = {}
    for k in keys:
        if k == "nab":
            shared[k] = np.stack([_na_bias_strips(inp["na_rpb"][jj]) for jj in range(2)], 0)
        else:
            shared[k] = np.ascontiguousarray(np.asarray(inp[k], np.float32))
    in_maps = []
    for b in range(cores):
        m = {"xin": state[b], "vecs": vec_arrs[b], "consts": consts}
        m.update(shared)
        in_maps.append(m)
    res = run_bass_kernel_spmd(nc, in_maps, core_ids=list(range(cores)), trace=trace)
    run_layers.last = res
    return [np.asarray(r["hout"], np.float32) for r in res.results]


def _initial_state(inp, b):
    x = np.asarray(inp["x"][b], np.float32)
    ctx = np.asarray(inp["ctx"][b], np.float32)
    return np.ascontiguousarray(np.concatenate([ctx, x], axis=0).T)


LAUNCH_GROUPS = [[0, 1, 2, 3]]


def kernel(**inputs):
    state = [_initial_state(inputs, b) for b in range(8)]
    for grp in LAUNCH_GROUPS:
        state = run_layers(inputs, state, grp)
    out = np.stack([s[:, NCTX:].T for s in state], axis=0)
    return np.ascontiguousarray(out.astype(np.float32))
```

```python
import numpy as np
from contextlib import ExitStack
import concourse.bass as bass
import concourse.mybir as mybir
from concourse.bass_utils import run_bass_kernel_spmd

F32 = mybir.dt.float32
BF16 = mybir.dt.bfloat16
AF = mybir.ActivationFunctionType
ALU = mybir.AluOpType
AX = mybir.AxisListType

ENGS = ("pe", "act", "dve", "pool", "sp")


class Op:
    __slots__ = ("eng", "fn", "deps", "idx", "signal", "is_dma", "semkey", "seq")

    def __init__(self, eng, fn, is_dma=False, semkey=None):
        self.eng = eng
        self.fn = fn
        self.deps = []
        self.signal = False
        self.is_dma = is_dma
        self.semkey = semkey
        self.seq = None


class Prog:
    def __init__(self, nc):
        self.nc = nc
        self.ops = {e: [] for e in ENGS}
        self.last_writer = {}
        self.readers = {}
        self._yield = None

    def op(self, eng, fn, reads=(), writes=(), dma=False, semkey=None):
        o = Op(eng, fn, is_dma=dma, semkey=semkey)
        deps = set()
        for r in reads:
            w = self.last_writer.get(r)
            if w is not None:
                deps.add(w)
        for w_ in writes:
            w = self.last_writer.get(w_)
            if w is not None:
                deps.add(w)
            for rd in self.readers.get(w_, ()):
                deps.add(rd)
        o.deps = list(deps)
        for r in reads:
            self.readers.setdefault(r, []).append(o)
        for w_ in writes:
            self.last_writer[w_] = o
            self.readers[w_] = []
        o.idx = len(self.ops[eng])
        self.ops[eng].append(o)
        if self._yield is not None:
            self._yield()
        return o

    def interleave(self, fns):
        import threading
        n = len(fns)
        batons = [threading.Semaphore(0) for _ in range(n)]
        done = [False] * n
        errs = []
        main = threading.Semaphore(0)
        tl = threading.local()

        def pass_baton(i):
            for k in range(1, n + 1):
                jn = (i + k) % n
                if not done[jn]:
                    batons[jn].release()
                    return
            main.release()

        def yielder():
            i = getattr(tl, "idx", None)
            if i is None:
                return
            pass_baton(i)
            batons[i].acquire()

        def runner(i):
            batons[i].acquire()
            tl.idx = i
            try:
                fns[i]()
            except BaseException as e:
                errs.append(e)
            done[i] = True
            pass_baton(i)

        ths = [threading.Thread(target=runner, args=(i,)) for i in range(n)]
        self._yield = yielder
        for t in ths:
            t.start()
        batons[0].release()
        main.acquire()
        for t in ths:
            t.join()
        self._yield = None
        if errs:
            raise errs[0]

    def barrier(self):
        lasts = [self.ops[e][-1] for e in ENGS if self.ops[e]]
        for e in ENGS:
            o = Op(e, lambda eng: eng.nop())
            o.deps = list(lasts)
            o.idx = len(self.ops[e])
            self.ops[e].append(o)

    def mm(self, out, lhsT, rhs, start, stop, reads, writes):
        return self.op("pe", lambda e: e.matmul(out, lhsT, rhs, start=start, stop=stop), reads, writes)

    def tr(self, out, in_, ident, reads, writes):
        return self.op("pe", lambda e: e.transpose(out, in_, ident), reads, writes)

    def dma(self, out, in_, reads, writes, eng="sp", semkey=None):
        return self.op(eng, lambda e: e.dma_start(out=out, in_=in_), reads, writes, dma=True, semkey=semkey)

    def act(self, out, in_, func, reads, writes, scale=None, bias=None):
        kw = {}
        if scale is not None:
            kw["scale"] = scale
        if bias is not None:
            kw["bias"] = bias
        return self.op("act", lambda e: e.activation(out=out, in_=in_, func=func, **kw), reads, writes)

    def tt(self, out, in0, in1, op, reads, writes, eng="dve"):
        return self.op(eng, lambda e: e.tensor_tensor(out=out, in0=in0, in1=in1, op=op), reads, writes)

    def ts(self, out, in0, s1, s2, op0, op1, reads, writes, eng="dve"):
        if op1 is None:
            return self.op(eng, lambda e: e.tensor_scalar(out=out, in0=in0, scalar1=s1, scalar2=None, op0=op0), reads, writes)
        return self.op(eng, lambda e: e.tensor_scalar(out=out, in0=in0, scalar1=s1, scalar2=s2, op0=op0, op1=op1), reads, writes)

    def stt(self, out, in0, scalar, in1, op0, op1, reads, writes):
        return self.op("dve", lambda e: e.scalar_tensor_tensor(out=out, in0=in0, scalar=scalar, in1=in1, op0=op0, op1=op1), reads, writes)

    def cp(self, out, in_, reads, writes, eng="dve"):
        if eng == "act":
            return self.op("act", lambda e: e.activation(out=out, in_=in_, func=AF.Copy), reads, writes)
        return self.op(eng, lambda e: e.tensor_copy(out=out, in_=in_), reads, writes)

    def recip(self, out, in_, reads, writes):
        return self.op("dve", lambda e: e.reciprocal(out=out, in_=in_), reads, writes)

    def scan(self, out, d0, d1, init, reads, writes):
        return self.op("dve", lambda e: e.tensor_tensor_scan(out=out, data0=d0, data1=d1, initial=init, op0=ALU.mult, op1=ALU.add), reads, writes)

    def memset(self, ap, val, reads, writes, eng="dve"):
        return self.op(eng, lambda e: e.memset(ap, val), reads, writes)

    def emit(self, block, stack):
        nc = self.nc
        for e in ENGS:
            for o in self.ops[e]:
                for d in o.deps:
                    if d.is_dma or d.eng != o.eng or o.eng != "pe":
                        d.signal = True
        sems = {}
        for e in ("pe", "act", "dve", "pool", "sp"):
            sems[e] = stack.enter_context(nc.semaphore("s_" + e))
        dma_counts = {}
        for e in ENGS:
            for o in self.ops[e]:
                if o.is_dma and o.semkey not in sems:
                    sems[o.semkey] = stack.enter_context(nc.semaphore("d_" + str(o.semkey)))
                    dma_counts[o.semkey] = 0
        self.n_sems = len(sems)
        for e in ENGS:
            cnt = 0
            for o in self.ops[e]:
                if o.is_dma:
                    dma_counts[o.semkey] += 16
                    o.seq = dma_counts[o.semkey]
                elif o.signal:
                    cnt += 1
                    o.seq = cnt

        def run_engine(ename, eng):
            water = {}
            for o in self.ops[ename]:
                need = {}
                for d in o.deps:
                    if d.is_dma:
                        key = d.semkey
                    else:
                        if d.eng == ename and ename == "pe":
                            continue
                        key = d.eng
                    if d.seq is None:
                        continue
                    if need.get(key, 0) < d.seq:
                        need[key] = d.seq
                for key, v in need.items():
                    if water.get(key, 0) < v:
                        eng.wait_ge(sems[key], v)
                        water[key] = v
                ins = o.fn(eng)
                if o.is_dma:
                    ins.then_inc(sems[o.semkey], 16)
                elif o.signal:
                    ins.then_inc(sems[ename], 1)

        block.tensor(lambda eng: run_engine("pe", eng))
        block.scalar(lambda eng: run_engine("act", eng))
        block.vector(lambda eng: run_engine("dve", eng))
        block.gpsimd(lambda eng: run_engine("pool", eng))
        block.sync(lambda eng: run_engine("sp", eng))


DM = 1024
NCTX = 256
SEQ = 2048
T = NCTX + SEQ
DEPTH = 4
TILES = [(0, 256), (256, 512), (768, 512), (1280, 512), (1792, 512)]
DFF = 2816
NFC = DFF // 128
NEXP = 8
EPS = 1e-6
EVEN_IN = 3584
GRID_W = 64
ROWS = 32
NEG = -30000.0
ARENA_N = 28672
FG = 2
NSLOT = 2


def _chunks(v):
    v = np.asarray(v, np.float32)
    return np.ascontiguousarray(v.reshape(-1, 128).T)


class _Packer:
    def __init__(self):
        self.cols = []
        self.off = {}
        self.n = 0

    def add(self, name, arr):
        arr = np.asarray(arr, np.float32).reshape(128, -1)
        self.off[name] = (self.n, arr.shape[1])
        self.cols.append(arr)
        self.n += arr.shape[1]

    def pack(self):
        return np.ascontiguousarray(np.concatenate(self.cols, axis=1))


def _vec_layout(inp, b):
    pk = _Packer()
    cc = np.stack([_chunks(inp["c"][b]), _chunks(inp["c_ctx"])], axis=-1)
    pk.add("cc", cc)
    for l in range(DEPTH):
        pk.add("bmod%d" % l, _chunks(inp["b_mod"][l]))
        pk.add("gmix%d" % l, _chunks(inp["norm_mix_g"][l]))
        pk.add("gffn%d" % l, _chunks(inp["norm_ffn_g"][l]))
    lbl = np.stack([np.stack([_chunks(inp["hg_lb_logits"][jj, d]) for d in range(2)], 1) for jj in range(2)], 1)
    pk.add("lbl", lbl)
    for j in range(2):
        pk.add("onorm%d" % j, _chunks(inp["hg_onorm_g"][j]))
        pk.add("convw%d" % j, np.stack([_chunks(inp["rg_conv_w"][j, k]) for k in range(4)], 1))
        pk.add("convb%d" % j, _chunks(inp["rg_conv_b"][j]))
        pk.add("ba%d" % j, np.stack([_chunks(inp["rg_ba"][j, d]) for d in range(2)], 1))
        pk.add("bx%d" % j, np.stack([_chunks(inp["rg_bx"][j, d]) for d in range(2)], 1))
        pk.add("lam%d" % j, np.stack([_chunks(inp["rg_lambda"][j, d]) for d in range(2)], 1))
        pk.add("qg%d" % j, np.tile(np.asarray(inp["na_q_g"][j], np.float32), 2).reshape(128, 1))
        pk.add("kg%d" % j, np.tile(np.asarray(inp["na_k_g"][j], np.float32), 2).reshape(128, 1))
        wr = np.asarray(inp["moe_router"][j], np.float32).reshape(8, 128, 8).transpose(1, 0, 2)
        pk.add("wr%d" % j, wr)
    return pk.off, pk.pack()


def _consts():
    p = np.arange(128)
    ident = (p[:, None] == p[None, :]).astype(np.float32)
    ones = np.ones((128, 128), np.float32)
    blk = ((p[:, None] // 64) == (p[None, :] // 64)).astype(np.float32)
    s = (p % 64)[:, None]
    t = np.arange(64)[None, :]
    mF = -(s <= t).astype(np.float32)
    mB = -(s >= t).astype(np.float32)
    cm = np.broadcast_to((np.arange(512) % 64 != 0).astype(np.float32)[None, :], (128, 512))
    bf_part = np.concatenate([ident, ones, blk, mF, mB, cm], axis=1)
    return np.ascontiguousarray(np.concatenate([ident, bf_part], axis=1).astype(np.float32))


def _na_rel_tables():
    pats = {}
    rel = {}
    plist = []
    for m in range(16):
        for j in range(16):
            q = np.arange(128)
            qr = 2 * m + q // 64
            qc = q % 64
            kr = 2 * j + q // 64
            kc = q % 64
            rs = np.clip(qr - 4, 0, ROWS - 8)
            rowv = (kr[None, :] >= rs[:, None]) & (kr[None, :] < rs[:, None] + 8)
            ws = np.clip(qc - 8, 0, GRID_W - 16)
            colv = (kc[None, :] >= ws[:, None]) & (kc[None, :] < ws[:, None] + 16)
            valid = rowv & colv
            if not valid.any():
                continue
            dr = np.clip(kr[None, :] - qr[:, None] + 7, 0, 14)
            dc = np.clip(kc[None, :] - qc[:, None] + 15, 0, 30)
            key = (valid.tobytes(), (dr * valid).tobytes(), (dc * valid).tobytes())
            if key not in pats:
                pats[key] = len(plist)
                plist.append((valid, dr, dc))
            rel[(m, j)] = pats[key]
    return rel, plist


_NA_REL, _NA_PATS = _na_rel_tables()
NREL = len(_NA_PATS)


def _na_strip_tables():
    js_of = {m: [jj for jj in range(16) if (m, jj) in _NA_REL] for m in range(16)}
    strips = {}
    strip_of = {}
    blocks = []
    for m in range(16):
        key = tuple(_NA_REL[(m, jj)] for jj in js_of[m])
        if key not in strips:
            strips[key] = (len(blocks), len(key))
            blocks.extend(key)
        strip_of[m] = strips[key]
    return js_of, strip_of, blocks


_NA_JS, _NA_STRIP, _NA_STRIP_RELS = _na_strip_tables()
NSTRIP_BLK = len(_NA_STRIP_RELS)


def _na_bias_strips(rpb):
    blk = _na_bias_blocks(rpb)
    return np.ascontiguousarray(blk[:, _NA_STRIP_RELS].transpose(0, 1, 3, 2))


def _na_bias_blocks(rpb):
    rpb = np.asarray(rpb, np.float32)
    out = np.empty((16, NREL, 128, 128), np.float32)
    for r, (valid, dr, dc) in enumerate(_NA_PATS):
        g = rpb[:, dr, dc]
        out[:, r] = np.where(valid[None], g, np.float32(NEG))
    return out


DEBUG_STOP = None
SEQ_E1 = False
DBG_A = True
DBG_B = True
DBG_C = 'dve'


def build(layers, voff, mixer=True, ffn=True):
    nc = bass.Bass("TRN2", target_bir_lowering=False)
    dram = {}

    def DR(name, shape, kind="ExternalInput"):
        if name not in dram:
            dram[name] = nc.dram_tensor(name, list(shape), F32, kind=kind).ap()
        return dram[name]

    xin = DR("xin", [DM, T])
    hout = DR("hout", [DM, T], kind="ExternalOutput")
    vecs_d = DR("vecs", [128, voff["_n"]])
    consts_d = DR("consts", [128, 1152])

    st = ExitStack()
    with st:
        def sb(name, shape, dt):
            return st.enter_context(nc.sbuf_tensor("sb_" + name, shape, dt))

        h = sb("h", [128, 8, T], F32)
        u = sb("u", [128, 8, T], BF16)
        M = sb("M", [128, 8, T], BF16)
        arena = sb("arena", [128, ARENA_N], BF16)
        vecs = sb("vecs", [128, voff["_n"]], F32)
        identf = sb("identf", [128, 128], F32)
        maskf = sb("maskf", [128, 128], F32)
        cbf = sb("cbf", [128, 1024], BF16)
        modv = sb("modv", [128, 48, 2], F32)
        mvec = sb("mvec", [128, 6, 8, 2], F32)
        scc = sb("scc", [128, 8, 2], BF16)
        misc = sb("misc", [128, 64], F32)
        pbs = [st.enter_context(nc.psum_tensor("pb%d" % i, [128, 512], F32)) for i in range(8)]

        ident_bf = cbf[:, 0:128]
        ones_bf = cbf[:, 128:256]
        blk_bf = cbf[:, 256:384]
        maskF = cbf[:, 384:448]
        maskB = cbf[:, 448:512]
        cmask = cbf[:, 512:1024]

        P = Prog(nc)

        def V(name, *idx):
            o, n = voff[name]
            return vecs[:, o:o + n]

        class Arena:
            def __init__(self):
                self.off = 0

            def bf(self, n, shape=None):
                n2 = (n + 15) // 16 * 16
                assert self.off + n2 <= ARENA_N, ("arena overflow", self.off, n2)
                ap = arena[:, self.off:self.off + n]
                self.off += n2
                return ap

            def f32(self, n):
                n2 = (2 * n + 15) // 16 * 16
                assert self.off + n2 <= ARENA_N, ("arena overflow", self.off, n2)
                ap = arena[:, self.off:self.off + 2 * n].bitcast(F32)
                self.off += n2
                return ap

        P.dma(vecs[:], vecs_d, [], ["vecs"], semkey="vecs")
        P.dma(identf[:], consts_d[:, 0:128], [], ["identf"], semkey="consts")
        P.dma(maskf[:], consts_d[:, 512:640], [], ["maskf"], semkey="maskf")
        P.dma(cbf[:], consts_d[:, 128:1152], [], ["cbf"], eng="pool", semkey="cbf")
        xin_v = xin.rearrange("(c p) t -> p c t", p=128)
        for c in range(8):
            P.dma(h[:, c, :], xin_v[:, c, :], [], [("h", c, ti) for ti in range(5)], semkey="hload")
        o_cc = voff["cc"][0]
        ccv = vecs[:, o_cc:o_cc + 16]
        P.act(scc[:].rearrange("p k two -> p (k two)"), ccv, AF.Silu, ["vecs"], ["scc"])

        def adaln(l):
            P.barrier()
            ar = Arena()
            slots = [ar.bf(8 * 1024).rearrange("p (k f) -> p k f", k=8) for _ in range(2)]
            wm = DR("w_mod", [DEPTH, DM, 6 * DM])
            mp = pbs[0][:, 0:96].rearrange("p (j two) -> p j two", two=2)
            for gi in range(6):
                sl = slots[gi % 2]
                P.dma(sl, wm[l][:, gi * 1024:(gi + 1) * 1024].rearrange("(k p) f -> p k f", p=128),
                      [], [("admw", gi % 2)], eng="pool", semkey="admw%d" % (gi % 2))
                for j in range(8):
                    jj = gi * 8 + j
                    for k in range(8):
                        P.mm(mp[:, jj, :], sl[:, k, j * 128:(j + 1) * 128], scc[:, k, :], k == 0, k == 7,
                             [("admw", gi % 2), "scc"], [("admp", jj)])
            ob = voff["bmod%d" % l][0]
            bm = vecs[:, ob:ob + 48]
            P.tt(modv[:], mp, bm.unsqueeze(2).to_broadcast([128, 48, 2]), ALU.add,
                 [("admp", jj) for jj in range(48)] + ["vecs"], ["modv"])
            for which, (gname, isc, ish, igt) in enumerate((("gmix%d" % l, 1, 0, 2), ("gffn%d" % l, 4, 3, 5))):
                og = voff[gname][0]
                gv = vecs[:, og:og + 8]
                Aq = mvec[:, which * 3 + 0]
                P.ts(Aq, modv[:, isc * 8:(isc + 1) * 8, :], 1.0, None, ALU.add, None, ["modv"], ["mvec"])
                P.tt(Aq, Aq, gv.unsqueeze(2).to_broadcast([128, 8, 2]), ALU.mult, ["mvec", "vecs"], ["mvec"])
                P.cp(mvec[:, which * 3 + 1], modv[:, ish * 8:(ish + 1) * 8, :], ["modv", "mvec"], ["mvec"])
                P.cp(mvec[:, which * 3 + 2], modv[:, igt * 8:(igt + 1) * 8, :], ["modv", "mvec"], ["mvec"])

        def modulate(l, which, tiles, router_j=None, rl_ps=None):
            P.barrier()
            ar = Arena()
            sq = ar.bf(8 * 512).rearrange("p (k n) -> p k n", k=8)
            rt = ar.f32(512)
            rstd = ar.f32(512)
            tmp = [ar.f32(512) for _ in range(2)]
            uf = [ar.f32(512) for _ in range(2)]
            Aq, Bq = mvec[:, which * 3 + 0], mvec[:, which * 3 + 1]
            n = 0
            for ti in tiles:
                t0, N = TILES[ti]
                col = 1 if ti == 0 else 0
                for c in range(8):
                    P.act(sq[:, c, 0:N], h[:, c, t0:t0 + N], AF.Square, [("h", c, ti)], [("sq", c)])
                for c in range(8):
                    P.mm(pbs[0][:, 0:N], ones_bf, sq[:, c, 0:N], c == 0, c == 7, [("sq", c), "cbf"], ["ssps"])
                P.act(rt[:, 0:N], pbs[0][:, 0:N], AF.Ln, ["ssps"], ["rt"], scale=1.0 / DM, bias=EPS)
                P.act(rstd[:, 0:N], rt[:, 0:N], AF.Exp, ["rt"], ["rstd"], scale=-0.5)
                for c in range(8):
                    tb = tmp[n % 2]
                    P.stt(tb[:, 0:N], h[:, c, t0:t0 + N], Aq[:, c, col:col + 1], rstd[:, 0:N], ALU.mult, ALU.mult,
                          [("h", c, ti), "rstd", "mvec"], [("tmp", n % 2)])
                    if router_j is None:
                        P.act(u[:, c, t0:t0 + N], tb[:, 0:N], AF.Identity, [("tmp", n % 2), "mvec"], [("u", c, ti)],
                              bias=Bq[:, c, col:col + 1])
                    else:
                        ub = uf[n % 2]
                        P.act(ub[:, 0:N], tb[:, 0:N], AF.Identity, [("tmp", n % 2), "mvec"], [("uf", n % 2)],
                              bias=Bq[:, c, col:col + 1])
                        P.cp(u[:, c, t0:t0 + N], ub[:, 0:N], [("uf", n % 2)], [("u", c, ti)], eng="pool")
                        ow = voff["wr%d" % router_j][0]
                        P.mm(rl_ps[ti][0:8, 0:N], vecs[:, ow + c * 8:ow + (c + 1) * 8], ub[:, 0:N], c == 0, c == 7,
                             [("uf", n % 2), "vecs"], [("rlps", ti)])
                    n += 1

        def ffn_phase(l, tiles, experts, gatesT=None):
            ar = Arena()
            FGM = 4
            slots = []
            for s in range(2):
                w1s = ar.bf(8 * FGM * 128).rearrange("p (k f) -> p k f", k=8)
                w3s = ar.bf(8 * FGM * 128).rearrange("p (k f) -> p k f", k=8)
                w2s = ar.bf(FGM * 1024).rearrange("p (g d) -> p g d", g=FGM)
                slots.append((w1s, w3s, w2s))
            mo = [4608]

            def mbf(n):
                ap = Mflat[:, mo[0]:mo[0] + n]
                mo[0] += n
                assert mo[0] <= 8 * T
                return ap

            def mf32(n):
                return mbf(2 * n).bitcast(F32)

            gbc = mf32(T)
            actb = [mbf(FGM * 512).rearrange("p (g n) -> p g n", g=FGM) for _ in range(2)]
            s1b = [mf32(512) for _ in range(2)]
            s1g = [mf32(512) for _ in range(2)]
            G2 = mvec[:, 5]
            jobs = []
            for (w1a, w3a, w2a, ei) in experts:
                f0 = 0
                while f0 < NFC:
                    fg = min(FGM, NFC - f0)
                    jobs.append((w1a, w3a, w2a, ei, f0, fg))
                    f0 += fg

            def issue(i):
                w1a, w3a, w2a, ei, f0, fg = jobs[i]
                s = i % 2
                w1s, w3s, w2s = slots[s]
                fs = slice(f0 * 128, (f0 + fg) * 128)
                P.dma(w1s[:, :, 0:fg * 128], w1a[:, fs].rearrange("(k p) f -> p k f", p=128), [], [("fw1", s)], eng="pool", semkey="fw1_%d" % s)
                P.dma(w3s[:, :, 0:fg * 128], w3a[:, fs].rearrange("(k p) f -> p k f", p=128), [], [("fw3", s)], eng="pool", semkey="fw3_%d" % s)
                P.dma(w2s[:, 0:fg, :], w2a[fs, :].rearrange("(g p) d -> p g d", p=128), [], [("fw2", s)], eng="pool", semkey="fw2_%d" % s)

            nt = 0
            ng = 0
            ny = 0
            ybanks = [pbs[4], pbs[5], pbs[7]]
            issue(0)
            for i, (w1a, w3a, w2a, ei, f0, fg) in enumerate(jobs):
                if i + 1 < len(jobs):
                    issue(i + 1)
                s = i % 2
                w1s, w3s, w2s = slots[s]
                for ti in tiles:
                    t0, N = TILES[ti]
                    col = 1 if ti == 0 else 0
                    if ei is not None and f0 == 0:
                        P.mm(pbs[6][:, 0:N], identf[0:8, ei:ei + 1].to_broadcast([8, 128]), gatesT[0:8, t0:t0 + N],
                             True, True, ["gatesT", "identf"], ["gbps"])
                        P.cp(gbc[:, t0:t0 + N], pbs[6][:, 0:N], ["gbps"], [("gbc", ti)], eng="act")
                    ab = actb[nt % 2]
                    for g in range(fg):
                        hp1 = pbs[0 + (ng % 2)]
                        hp3 = pbs[2 + (ng % 2)]
                        for k in range(8):
                            P.mm(hp1[:, 0:N], w1s[:, k, g * 128:(g + 1) * 128], u[:, k, t0:t0 + N], k == 0, k == 7,
                                 [("fw1", s), ("u", k, ti)], [("hp1", ng % 2)])
                        for k in range(8):
                            P.mm(hp3[:, 0:N], w3s[:, k, g * 128:(g + 1) * 128], u[:, k, t0:t0 + N], k == 0, k == 7,
                                 [("fw3", s), ("u", k, ti)], [("hp3", ng % 2)])
                        sb1 = s1b[ng % 2]
                        P.act(sb1[:, 0:N], hp1[:, 0:N], AF.Silu, [("hp1", ng % 2)], [("s1", ng % 2)])
                        if ei is not None:
                            sg = s1g[ng % 2]
                            P.tt(sg[:, 0:N], hp3[:, 0:N], gbc[:, t0:t0 + N], ALU.mult, [("hp3", ng % 2), ("gbc", ti)], [("s1g", ng % 2)])
                            P.tt(ab[:, g, 0:N], sg[:, 0:N], sb1[:, 0:N], ALU.mult, [("s1g", ng % 2), ("s1", ng % 2)], [("act", nt % 2, g)])
                        else:
                            P.tt(ab[:, g, 0:N], sb1[:, 0:N], hp3[:, 0:N], ALU.mult, [("s1", ng % 2), ("hp3", ng % 2)],
                                 [("act", nt % 2, g)])
                        ng += 1
                    for dc in range(8):
                        yp = ybanks[ny % 3]
                        for g in range(fg):
                            P.mm(yp[:, 0:N], w2s[:, g, dc * 128:(dc + 1) * 128], ab[:, g, 0:N], g == 0, g == fg - 1,
                                 [("fw2", s), ("act", nt % 2, g)], [("yp", ny % 3)])
                        P.stt(h[:, dc, t0:t0 + N], yp[:, 0:N], G2[:, dc, col:col + 1], h[:, dc, t0:t0 + N],
                              ALU.mult, ALU.add, [("yp", ny % 3), ("h", dc, ti), "mvec"], [("h", dc, ti)])
                        ny += 1
                    nt += 1

        def dense_ffn(l, tiles):
            j = l // 2
            w1 = DR("ffn_w1", [2, DM, DFF])
            w3 = DR("ffn_w3", [2, DM, DFF])
            w2 = DR("ffn_w2", [2, DFF, DM])
            modulate(l, 1, tiles)
            P.barrier()
            ffn_phase(l, tiles, [(w1[j], w3[j], w2[j], None)])

        def moe_ffn(l, tiles):
            j = l // 2
            w1 = DR("moe_w1", [2, NEXP, DM, DFF])
            w3 = DR("moe_w3", [2, NEXP, DM, DFF])
            w2 = DR("moe_w2", [2, NEXP, DFF, DM])
            rl_ps = {ti: pbs[1 + ti] for ti in range(5)}
            modulate(l, 1, tiles, router_j=j, rl_ps=rl_ps)
            P.barrier()
            ar = Arena()
            lgT = ar.f32(T)
            ntile = 18
            L = ar.f32(ntile * 8).rearrange("p (a e) -> p a e", e=8)
            eq1 = ar.f32(ntile * 8).rearrange("p (a e) -> p a e", e=8)
            eq2 = ar.f32(ntile * 8).rearrange("p (a e) -> p a e", e=8)
            L2 = ar.f32(ntile * 8).rearrange("p (a e) -> p a e", e=8)
            gts = ar.f32(ntile * 8).rearrange("p (a e) -> p a e", e=8)
            m1 = ar.f32(ntile)
            m2 = ar.f32(ntile)
            dd = ar.f32(ntile)
            w1v = ar.f32(ntile)
            w2v = ar.f32(ntile)
            for ti in tiles:
                t0, N = TILES[ti]
                P.cp(lgT[0:8, t0:t0 + N], rl_ps[ti][0:8, 0:N], [("rlps", ti)], [("lgT", ti)], eng="act")
            a0 = 0 if 0 in tiles else 2
            LP = pbs[0][:, 0:ntile * 8].rearrange("p (a e) -> p a e", e=8)
            for a in range(a0, ntile):
                ti = 0 if a < 2 else 1 + (a - 2) // 4
                P.tr(LP[:, a, :], lgT[0:8, a * 128:(a + 1) * 128], identf[0:8, 0:8], [("lgT", ti), "identf"], [("LP", a)])
            rd = [("LP", a) for a in range(a0, ntile)]
            sl = slice(a0, ntile)
            na = ntile - a0
            P.cp(L[:, sl, :], LP[:, sl, :], rd, ["L"])
            P.op("dve", lambda e: e.tensor_reduce(out=m1[:, sl], in_=L[:, sl, :], axis=AX.X, op=ALU.max), ["L"], ["m1"])
            P.tt(eq1[:, sl, :], L[:, sl, :], m1[:, sl].unsqueeze(2).to_broadcast([128, na, 8]), ALU.is_equal, ["L", "m1"], ["eq1"])
            P.stt(L2[:, sl, :], eq1[:, sl, :], -1e30, L[:, sl, :], ALU.mult, ALU.add, ["eq1", "L"], ["L2"])
            P.op("dve", lambda e: e.tensor_reduce(out=m2[:, sl], in_=L2[:, sl, :], axis=AX.X, op=ALU.max), ["L2"], ["m2"])
            P.tt(eq2[:, sl, :], L2[:, sl, :], m2[:, sl].unsqueeze(2).to_broadcast([128, na, 8]), ALU.is_equal, ["L2", "m2"], ["eq2"])
            P.tt(dd[:, sl], m2[:, sl], m1[:, sl], ALU.subtract, ["m1", "m2"], ["dd"])
            P.act(dd[:, sl], dd[:, sl], AF.Exp, ["dd"], ["dd"])
            P.ts(w1v[:, sl], dd[:, sl], 1.0, None, ALU.add, None, ["dd"], ["w1v"])
            P.recip(w1v[:, sl], w1v[:, sl], ["w1v"], ["w1v"])
            P.tt(w2v[:, sl], dd[:, sl], w1v[:, sl], ALU.mult, ["dd", "w1v"], ["w2v"])
            P.tt(gts[:, sl, :], eq1[:, sl, :], w1v[:, sl].unsqueeze(2).to_broadcast([128, na, 8]), ALU.mult, ["eq1", "w1v"], ["gts"])
            P.tt(eq2[:, sl, :], eq2[:, sl, :], w2v[:, sl].unsqueeze(2).to_broadcast([128, na, 8]), ALU.mult, ["eq2", "w2v"], ["eq2"])
            P.tt(gts[:, sl, :], gts[:, sl, :], eq2[:, sl, :], ALU.add, ["gts", "eq2"], ["gts"])
            gatesT = misc_big["gatesT"]
            for a in range(a0, ntile):
                gp = pbs[1 + (a % 2)]
                P.tr(gp[0:8, 0:128], gts[:, a, :], identf[:, :], ["gts", "identf"], [("gp", a % 2)])
                P.cp(gatesT[0:8, a * 128:(a + 1) * 128], gp[0:8, 0:128], [("gp", a % 2)], ["gatesT"], eng="act")
            P.barrier()
            ffn_phase(l, tiles, [(w1[j, e], w3[j, e], w2[j, e], e) for e in range(NEXP)], gatesT=gatesT)

        Mflat = M[:, :, :].rearrange("p a t -> p (a t)")
        misc_big = {"gatesT": Mflat[:, 0:4608].bitcast(F32)}

        def out_proj(l, w_ap):
            ar = Arena()
            wo = ar.bf(8 * 1024).rearrange("p (k f) -> p k f", k=8)
            P.dma(wo, w_ap.rearrange("(k p) f -> p k f", p=128), [], ["wo"], eng="pool", semkey="wo")
            G1 = mvec[:, 2]
            n = 0
            for ti in range(5):
                t0, N = TILES[ti]
                col = 1 if ti == 0 else 0
                for dc in range(8):
                    yp = pbs[n % 2]
                    for k in range(8):
                        P.mm(yp[:, 0:N], wo[:, k, dc * 128:(dc + 1) * 128], M[:, k, t0:t0 + N], k == 0, k == 7,
                             ["wo", ("M", k, ti)], [("yp", n % 2)])
                    P.stt(h[:, dc, t0:t0 + N], yp[:, 0:N], G1[:, dc, col:col + 1], h[:, dc, t0:t0 + N], ALU.mult, ALU.add,
                          [("yp", n % 2), ("h", dc, ti), "mvec"], [("h", dc, ti)])
                    n += 1

        def even_mixer(l):
            j = l // 2
            w_in = DR("ev_w_in", [2, DM, EVEN_IN])[j]
            w_out = DR("ev_w_out", [2, DM, DM])[j]
            wa_d = DR("rg_wa", [2, 2, 8, 64, 64])
            wx_d = DR("rg_wx", [2, 2, 8, 64, 64])

            def wcols(dst, c0, n):
                return w_in[:, c0:c0 + n].rearrange("(k p) f -> p k f", p=128)

            P.barrier()
            ar = Arena()
            Vall = M[:, 4:8, :].rearrange("p a t -> p (a t)").rearrange("p (i f) -> p i f", f=512)
            wi = ar.bf(8 * 512).rearrange("p (k f) -> p k f", k=8)
            P.dma(wi, wcols(wi, 3 * 512, 512), [], ["wi"], eng="pool", semkey="wi")
            for a in range(18):
                ti = 0 if a < 2 else 1 + (a - 2) // 4
                vp = pbs[a % 2]
                for k in range(8):
                    P.mm(vp[:, :], u[:, k, a * 128:(a + 1) * 128], wi[:, k, :], k == 0, k == 7, [("u", k, ti), "wi"], [("vp", a % 2)])
                P.cp(Vall[:, a, :], vp[:, :], [("vp", a % 2)], [("V", a)], eng="act" if a % 2 else "dve")
            for hd in range(4):
                for ti in range(5):
                    t0, N = TILES[ti]
                    P.memset(M[:, hd, t0:t0 + N], 0.0, [], [("M", hd, ti)], eng=DBG_C)
            P.barrier()
            ar = Arena()
            lbv = ar.f32(8)
            lbv3 = lbv.rearrange("p (d c) -> p d c", d=2)
            if j == 0:
                P.memset(lbv, 0.0, [], ["lbv"])
            else:
                ol = voff["lbl"][0]
                l0 = vecs[:, ol:ol + 8]
                l1 = vecs[:, ol + 8:ol + 16]
                P.tt(lbv, l0, l1, ALU.subtract, ["vecs"], ["lbv"])
                P.act(lbv, lbv, AF.Exp, ["lbv"], ["lbv"])
                P.ts(lbv, lbv, 1.0, None, ALU.add, None, ["lbv"], ["lbv"])
                P.recip(lbv, lbv, ["lbv"], ["lbv"])

            def e1_chain(d):
                cn = "c%d" % d
                wz = ar.bf(8 * 128).rearrange("p (k f) -> p k f", k=8)
                wq = ar.bf(8 * 128).rearrange("p (k f) -> p k f", k=8)
                X = [ar.f32(512) for _ in range(5)]
                SQf = ar.bf(T)
                qt, kt, kh = ar.bf(512), ar.bf(512), ar.bf(512)
                KT = ar.bf(512).rearrange("p (a k) -> p a k", k=128)
                AT = ar.bf(256).rearrange("p (a s) -> p a s", s=64)
                AT32 = ar.f32(256).rearrange("p (a s) -> p a s", s=64)
                e1v, e12v = ar.f32(8), ar.f32(8)
                Sb = [ar.f32(128) for _ in range(2)]
                Tb = [ar.bf(128) for _ in range(2)]
                pz, pat, pop, pap = pbs[4 * d + 0], pbs[4 * d + 1], pbs[4 * d + 2], pbs[4 * d + 3]
                bA, bB, bC, bD = (cn, "bA"), (cn, "bB"), (cn, "bC"), (cn, "bD")
                trp = pz[:, 0:256].bitcast(BF16)
                P.memset(AT32, 0.0, [], [(cn, "AT32")], eng="pool")
                order = [0, 1, 2, 3, 4] if d == 0 else [0, 4, 3, 2, 1]
                r_idx = 31 if d == 0 else 32
                l_idx = 63 if d == 0 else 0
                msk = (maskf[:, 0:64] if d == 0 else maskf[:, 64:128]).bitcast(mybir.dt.uint32)
                for hd in range(4):
                    P.dma(wz, wcols(wz, 512 * (1 + d) + hd * 128, 128), [], [(cn, "wz")], eng="pool", semkey="wz%d" % d)
                    P.dma(wq, wcols(wq, hd * 128, 128), [], [(cn, "wq")], eng="pool", semkey="wq%d" % d)
                    for ti in range(5):
                        t0, N = TILES[ti]
                        for k in range(8):
                            P.mm(pz[:, 0:N], wq[:, k, :], u[:, k, t0:t0 + N], k == 0, k == 7, [(cn, "wq"), ("u", k, ti)], [bA])
                        P.act(SQf[:, t0:t0 + N], pz[:, 0:N], AF.Silu, [bA], [(cn, "SQ", ti)])
                    Sc = 0
                    P.memset(Sb[0], 0.0, [], [(cn, "S", 0)])
                    for ti in order:
                        t0, N = TILES[ti]
                        nch = N // 64
                        na = N // 128
                        for k in range(8):
                            P.mm(pz[:, 0:N], wz[:, k, :], u[:, k, t0:t0 + N], k == 0, k == 7, [(cn, "wz"), ("u", k, ti)], [bA])
                        E, L1, L2, Fv, DL = [x[:, 0:N] for x in X]
                        xn = [(cn, "X%d" % i) for i in range(5)]
                        P.act(E, pz[:, 0:N], AF.Exp, [bA], [xn[0]], scale=-1.0)
                        P.act(L1, E, AF.Ln, [xn[0]], [xn[1]], bias=1.0)
                        P.act(L2, E, AF.Ln, [xn[0], "lbv"], [xn[2]], bias=1.0, scale=lbv3[:, d, hd:hd + 1])
                        P.tt(L2, L2, L1, ALU.subtract, [xn[2], xn[1]], [xn[2]])
                        P.act(Fv, L2, AF.Exp, [xn[2]], [xn[3]])
                        Bv = E
                        if d == 0:
                            P.scan(Bv, cmask[:, 0:N], L2, 0.0, [xn[2], "cbf", xn[0]], [xn[0]])
                        else:
                            P.scan(Bv[:, ::-1], cmask[:, 0:N], L2[:, ::-1], 0.0, [xn[2], "cbf", xn[0]], [xn[0]])
                        B3 = Bv.rearrange("p (a b) -> p a b", b=64)
                        Dv = L1
                        D3 = Dv.rearrange("p (a b) -> p a b", b=64)
                        P.tt(D3, B3, B3[:, :, r_idx:r_idx + 1].to_broadcast([128, nch, 64]), ALU.subtract, [xn[0], xn[1]], [xn[1]])
                        DL3 = DL.rearrange("p (a b) -> p a b", b=64)
                        P.tt(DL3, B3[:, :, l_idx:l_idx + 1].to_broadcast([128, nch, 64]), B3, ALU.subtract, [xn[0]], [xn[4]])
                        EK = L2
                        P.act(EK, Dv, AF.Exp, [xn[1], xn[2]], [xn[2]], scale=-1.0)
                        P.act(Dv, Dv, AF.Exp, [xn[1]], [xn[1]])
                        P.act(DL, DL, AF.Exp, [xn[4]], [xn[4]])
                        P.act(e1v[:, 0:nch], B3[:, :, r_idx], AF.Exp, [xn[0], (cn, "e")], [(cn, "e")])
                        P.act(e12v[:, 0:nch], B3[:, :, l_idx], AF.Exp, [xn[0], (cn, "e")], [(cn, "e")])
                        P.tt(qt[:, 0:N], SQf[:, t0:t0 + N], Dv, ALU.mult, [(cn, "SQ", ti), xn[1], (cn, "qt")], [(cn, "qt")])
                        P.stt(kt[:, 0:N], Fv, -1.0, EK, ALU.add, ALU.mult, [xn[3], xn[2], (cn, "kt")], [(cn, "kt")])
                        P.stt(kh[:, 0:N], Fv, -1.0, DL, ALU.add, ALU.mult, [xn[3], xn[4], (cn, "kh")], [(cn, "kh")])
                        for a in range(na):
                            P.tr(trp[:, a * 128:(a + 1) * 128], kh[:, a * 128:(a + 1) * 128], ident_bf, [(cn, "kh"), "cbf"], [bA])
                        P.cp(KT[:, 0:na, :].rearrange("p a k -> p (a k)"), trp[:, 0:N], [bA, (cn, "KT")], [(cn, "KT")], eng="dve")
                        atp = pat[:, 0:na * 64].rearrange("p (a s) -> p a s", s=64)
                        for jc in range(nch):
                            a, hf = jc // 2, jc % 2
                            P.mm(atp[hf * 64:(hf + 1) * 64, a, :], kt[:, jc * 64:(jc + 1) * 64], qt[:, jc * 64:(jc + 1) * 64],
                                 True, True, [(cn, "kt"), (cn, "qt")], [bB])
                        for a in range(na):
                            P.op("dve", lambda e, o_=AT32[:, a, :], m_=msk, d_=atp[:, a, :]: e.copy_predicated(out=o_, mask=m_, data=d_),
                                 [bB, "maskf", (cn, "AT32")], [(cn, "AT32")])
                        P.cp(AT[:, 0:na, :], AT32[:, 0:na, :], [(cn, "AT32"), (cn, "AT")], [(cn, "AT")], eng="act")
                        corder = list(range(nch)) if d == 0 else list(range(nch - 1, -1, -1))

                        def ap_of(pos):
                            jc_ = corder[pos]
                            bank, nm = (pap, bD) if jc_ % 2 == 0 else (pat, bB)
                            return bank[:, (jc_ // 2) * 128:(jc_ // 2 + 1) * 128], nm

                        def issue_ap(pos):
                            jc = corder[pos]
                            a, hf = jc // 2, jc % 2
                            ga = (t0 // 128) + a
                            hs = slice(hf * 64, (hf + 1) * 64)
                            Ap, nm = ap_of(pos)
                            P.mm(Ap, KT[hs, a, :], Vall[hs, ga, hd * 128:(hd + 1) * 128], True, True, [(cn, "KT"), ("V", ga)], [nm])

                        for pos in range(nch):
                            issue_ap(pos)
                        for pos, jc in enumerate(corder):
                            a, hf = jc // 2, jc % 2
                            ga = (t0 // 128) + a
                            hs = slice(hf * 64, (hf + 1) * 64)
                            cs = slice(jc * 64, (jc + 1) * 64)
                            Vh = Vall[hs, ga, hd * 128:(hd + 1) * 128]
                            Ap, nm = ap_of(pos)
                            S_old, S_new = Sb[Sc % 2], Sb[(Sc + 1) % 2]
                            tb = Tb[Sc % 2]
                            P.act(tb, S_old, AF.Copy, [(cn, "S", Sc % 2), (cn, "e")], [(cn, "Tb", Sc % 2)], scale=e1v[:, jc:jc + 1])
                            P.mm(pop[:, cs], tb, qt[:, cs], True, False, [(cn, "Tb", Sc % 2), (cn, "qt")], [bC])
                            P.mm(pop[:, cs], Vh, AT[hs, a, :], False, True, [(cn, "AT"), ("V", ga)], [bC])
                            P.stt(S_new, S_old, e12v[:, jc:jc + 1], Ap, ALU.mult, ALU.add,
                                  [(cn, "S", Sc % 2), nm, (cn, "e")], [(cn, "S", (Sc + 1) % 2)])
                            Sc += 1
                        P.tt(M[:, hd, t0:t0 + N], M[:, hd, t0:t0 + N], pop[:, 0:N], ALU.subtract, [bC, ("M", hd, ti)], [("M", hd, ti)])

            if SEQ_E1:
                e1_chain(0)
                e1_chain(1)
            else:
                P.interleave([lambda: e1_chain(0), lambda: e1_chain(1)])

            if DEBUG_STOP == 'E1':
                return 'dumpM'
            P.barrier()
            ar = Arena()
            cw_o = voff["convw%d" % j][0]
            cb_o = voff["convb%d" % j][0]
            ba_o = voff["ba%d" % j][0]
            bx_o = voff["bx%d" % j][0]
            lam_o = voff["lam%d" % j][0]

            def e2_chain(ci):
                cn = "r%d" % ci
                xp = ar.bf(T)
                hf = ar.bf(T)
                wxy = [ar.bf(8 * 128).rearrange("p (k f) -> p k f", k=8) for _ in range(2)]
                bd = [[ar.bf(128) for _ in range(2)] for _ in range(2)]
                xr, rr, ig, a2, Aat, BCt = [ar.f32(512) for _ in range(6)]
                xrb = ar.bf(512)
                sv = ar.f32(4).rearrange("p (d two) -> p d two", d=2)
                hbv = ar.f32(4).rearrange("p (d two) -> p d two", d=2)
                carry = ar.f32(1)
                pp, rp, ip = pbs[3 * ci], pbs[3 * ci + 1], pbs[3 * ci + 2]
                N_ = lambda x: (cn, x)
                for c in (2 * ci, 2 * ci + 1):
                    P.dma(wxy[0], wcols(None, 2560 + c * 128, 128), [], [N_("wx")], eng="pool", semkey="wx%d" % ci)
                    P.dma(wxy[1], wcols(None, 3072 + c * 128, 128), [], [N_("wy")], eng="pool", semkey="wy%d" % ci)
                    for d in range(2):
                        for mi, wd in enumerate((wa_d, wx_d)):
                            P.memset(bd[d][mi], 0.0, [], [N_(("bd", d, mi))], eng="pool")
                            for hb in range(2):
                                P.dma(bd[d][mi][hb * 64:(hb + 1) * 64, hb * 64:(hb + 1) * 64], wd[j, d, 2 * c + hb],
                                      [N_(("bd", d, mi))], [N_(("bd", d, mi))], eng="pool", semkey="bd%d%d%d" % (ci, d, mi))
                        lamv = vecs[:, lam_o + d * 4 + c:lam_o + d * 4 + c + 1]
                        P.act(sv[:, d, 0:1], lamv, AF.Exp, ["vecs"], [N_("sv")], scale=-1.0)
                        P.act(sv[:, d, 0:1], sv[:, d, 0:1], AF.Ln, [N_("sv")], [N_("sv")], bias=1.0)
                        P.ts(sv[:, d, 1:2], sv[:, d, 0:1], -8.0, None, ALU.mult, None, [N_("sv")], [N_("sv")])
                        P.ts(sv[:, d, 0:1], sv[:, d, 0:1], -4.0, None, ALU.mult, None, [N_("sv")], [N_("sv")])
                        P.ts(hbv[:, d, 0:1], vecs[:, ba_o + d * 4 + c:ba_o + d * 4 + c + 1], 0.5, None, ALU.mult, None, ["vecs"], [N_("hbv")])
                        P.ts(hbv[:, d, 1:2], vecs[:, bx_o + d * 4 + c:bx_o + d * 4 + c + 1], 0.5, None, ALU.mult, None, ["vecs"], [N_("hbv")])
                    for ti in range(5):
                        t0, N = TILES[ti]
                        for k in range(8):
                            P.mm(pp[:, 0:N], wxy[0][:, k, :], u[:, k, t0:t0 + N], k == 0, k == 7, [N_("wx"), ("u", k, ti)], [N_("pp")])
                        P.cp(xp[:, t0:t0 + N], pp[:, 0:N], [N_("pp")], [N_("xp")], eng="act")
                    for d in range(2):
                        order = [0, 1, 2, 3, 4] if d == 0 else [0, 4, 3, 2, 1]
                        firstt = True
                        for ti in order:
                            t0, N = TILES[ti]
                            s0, s1 = (0, NCTX) if ti == 0 else (NCTX, T)
                            xv = xr[:, 0:N]
                            P.ts(xv, xp[:, t0:t0 + N], vecs[:, cw_o + 2 * 4 + c:cw_o + 2 * 4 + c + 1], vecs[:, cb_o + c:cb_o + c + 1],
                                 ALU.mult, ALU.add, [N_("xp"), "vecs"], [N_("xr")])
                            for tap, sh in ((0, -2), (1, -1), (3, 1)):
                                lo = max(t0, s0 - sh)
                                hi = min(t0 + N, s1 - sh)
                                wv = vecs[:, cw_o + tap * 4 + c:cw_o + tap * 4 + c + 1]
                                P.stt(xr[:, lo - t0:hi - t0], xp[:, lo + sh:hi + sh], wv, xr[:, lo - t0:hi - t0], ALU.mult, ALU.add,
                                      [N_("xp"), N_("xr"), "vecs"], [N_("xr")])
                            P.cp(xrb[:, 0:N], xv, [N_("xr")], [N_("xrb")], eng="pool")
                            P.mm(rp[:, 0:N], bd[d][0], xrb[:, 0:N], True, True, [N_(("bd", d, 0)), N_("xrb")], [N_("rp")])
                            P.mm(ip[:, 0:N], bd[d][1], xrb[:, 0:N], True, True, [N_(("bd", d, 1)), N_("xrb")], [N_("ip")])
                            P.act(rr[:, 0:N], rp[:, 0:N], AF.Tanh, [N_("rp"), N_("hbv")], [N_("rr")], scale=0.5, bias=hbv[:, d, 0:1])
                            P.act(ig[:, 0:N], ip[:, 0:N], AF.Tanh, [N_("ip"), N_("hbv")], [N_("ig")], scale=0.5, bias=hbv[:, d, 1:2])
                            P.act(Aat[:, 0:N], rr[:, 0:N], AF.Exp, [N_("rr"), N_("sv")], [N_("Aat")], scale=sv[:, d, 0:1], bias=sv[:, d, 0:1])
                            P.act(a2[:, 0:N], rr[:, 0:N], AF.Exp, [N_("rr"), N_("sv")], [N_("a2")], scale=sv[:, d, 1:2], bias=sv[:, d, 1:2])
                            P.ts(a2[:, 0:N], a2[:, 0:N], -1.0, 1.0, ALU.mult, ALU.add, [N_("a2")], [N_("a2")])
                            P.ts(a2[:, 0:N], a2[:, 0:N], 1e-24, None, ALU.max, None, [N_("a2")], [N_("a2")])
                            P.act(a2[:, 0:N], a2[:, 0:N], AF.Sqrt, [N_("a2")], [N_("a2")])
                            P.stt(ig[:, 0:N], ig[:, 0:N], 1.0, xv, ALU.add, ALU.mult, [N_("ig"), N_("xr")], [N_("ig")])
                            P.stt(BCt[:, 0:N], a2[:, 0:N], 0.5, ig[:, 0:N], ALU.mult, ALU.mult, [N_("a2"), N_("ig")], [N_("BCt")])
                            init = 0.0 if firstt else carry[:, 0:1]
                            if d == 0:
                                P.scan(BCt[:, 0:N], Aat[:, 0:N], BCt[:, 0:N], init, [N_("Aat"), N_("BCt"), N_("carry")], [N_("BCt")])
                                P.cp(carry[:, 0:1], BCt[:, N - 1:N], [N_("BCt")], [N_("carry")], eng="dve")
                                P.cp(hf[:, t0:t0 + N], BCt[:, 0:N], [N_("BCt")], [N_(("hf", ti))], eng="pool")
                            else:
                                P.scan(BCt[:, N - 1::-1], Aat[:, N - 1::-1], BCt[:, N - 1::-1], init, [N_("Aat"), N_("BCt"), N_("carry")], [N_("BCt")])
                                P.cp(carry[:, 0:1], BCt[:, 0:1], [N_("BCt")], [N_("carry")], eng="dve")
                                for k in range(8):
                                    P.mm(pp[:, 0:N], wxy[1][:, k, :], u[:, k, t0:t0 + N], k == 0, k == 7, [N_("wy"), ("u", k, ti)], [N_("pp")])
                                yv, tv = rr[:, 0:N], xr[:, 0:N]
                                P.cp(yv, pp[:, 0:N], [N_("pp")], [N_("rr")], eng="act")
                                P.tt(tv, yv, yv, ALU.mult, [N_("rr")], [N_("xr")])
                                P.ts(tv, tv, 0.044715, 1.0, ALU.mult, ALU.add, [N_("xr")], [N_("xr")])
                                P.tt(tv, tv, yv, ALU.mult, [N_("xr"), N_("rr")], [N_("xr")])
                                P.act(tv, tv, AF.Tanh, [N_("xr")], [N_("xr")], scale=0.7978845608028654)
                                P.stt(tv, tv, 1.0, yv, ALU.add, ALU.mult, [N_("xr"), N_("rr")], [N_("xr")])
                                P.tt(yv, hf[:, t0:t0 + N], BCt[:, 0:N], ALU.add, [N_(("hf", ti)), N_("BCt")], [N_("rr")])
                                P.stt(M[:, 4 + c, t0:t0 + N], yv, 0.5, tv, ALU.mult, ALU.mult, [N_("rr"), N_("xr")], [("M", 4 + c, ti)])
                            firstt = False

            P.interleave([lambda: e2_chain(0), lambda: e2_chain(1)])
            if DEBUG_STOP == 'E2':
                return 'dumpM'
            P.barrier()
            ar = Arena()
            wg = ar.bf(8 * 512).rearrange("p (k f) -> p k f", k=8)
            sq = ar.bf(4 * 512).rearrange("p (k n) -> p k n", k=4)
            rt = ar.f32(512)
            rstd = ar.f32(512)
            sg = [ar.f32(512) for _ in range(2)]
            t1 = [ar.f32(512) for _ in range(2)]
            on_o = voff["onorm%d" % j][0]
            P.dma(wg, wcols(None, 2048, 512), [], ["wg"], eng="pool", semkey="wg")
            n = 0
            for ti in range(5):
                t0, N = TILES[ti]
                for c in range(4):
                    P.act(sq[:, c, 0:N], M[:, c, t0:t0 + N], AF.Square, [("M", c, ti)], [("sq", c)])
                for c in range(4):
                    P.mm(pbs[0][:, 0:N], ones_bf, sq[:, c, 0:N], c == 0, c == 3, [("sq", c), "cbf"], ["ssps"])
                P.act(rt[:, 0:N], pbs[0][:, 0:N], AF.Ln, ["ssps"], ["rt"], scale=1.0 / 512, bias=EPS)
                P.act(rstd[:, 0:N], rt[:, 0:N], AF.Exp, ["rt"], ["rstd"], scale=-0.5)
                for c in range(4):
                    gp = pbs[1 + n % 2]
                    for k in range(8):
                        P.mm(gp[:, 0:N], wg[:, k, c * 128:(c + 1) * 128], u[:, k, t0:t0 + N], k == 0, k == 7, ["wg", ("u", k, ti)], [("gp", n % 2)])
                    P.act(sg[n % 2][:, 0:N], gp[:, 0:N], AF.Silu, [("gp", n % 2)], [("sg", n % 2)])
                    P.stt(t1[n % 2][:, 0:N], M[:, c, t0:t0 + N], vecs[:, on_o + c:on_o + c + 1], rstd[:, 0:N], ALU.mult, ALU.mult,
                          [("M", c, ti), "rstd", "vecs"], [("t1", n % 2)])
                    P.tt(M[:, c, t0:t0 + N], t1[n % 2][:, 0:N], sg[n % 2][:, 0:N], ALU.mult, [("t1", n % 2), ("sg", n % 2), ("sq", c)], [("M", c, ti)])
                    n += 1
            if DEBUG_STOP == 'E3':
                return 'dumpM'
            P.barrier()
            out_proj(l, w_out)

        def na_mixer(l, with_ctx):
            j = l // 2
            wqkv = DR("na_w_qkv", [2, DM, 3 * DM])[j]
            w_o = DR("na_w_o", [2, DM, DM])[j]
            nab = DR("nab", [2, 16, NSTRIP_BLK, 128, 128])[j]
            P.barrier()
            ar = Arena()
            Qc = ar.bf(T)
            Kc = ar.bf(T)
            VA = ar.bf(18 * 130).rearrange("p (a f) -> p a f", f=130)
            w3 = [ar.bf(8 * 128).rearrange("p (k f) -> p k f", k=8) for _ in range(3)]
            bias = ar.bf(2 * NSTRIP_BLK * 128).rearrange("p (r q) -> p r q", q=128)
            sqb = [ar.bf(512) for _ in range(2)]
            rt = [ar.f32(512) for _ in range(2)]
            rstd = [ar.f32(512) for _ in range(2)]
            PT = [[ar.bf(512) for _ in range(2)] for _ in range(2)]
            OTt = [[ar.bf(128) for _ in range(2)] for _ in range(2)]
            rec = [[ar.f32(2) for _ in range(2)] for _ in range(2)]
            gs = ar.f32(2)
            P.ts(gs[:, 0:1], V("qg%d" % j), 0.125, None, ALU.mult, None, ["vecs"], ["gs"])
            P.cp(gs[:, 1:2], V("kg%d" % j), ["vecs", "gs"], ["gs"])
            VA4 = VA.rearrange("p a (h e) -> p a h e", h=2)
            P.memset(VA4[:, :, :, 64:65], 1.0, [], ["VAones"], eng="pool")
            for h_ in range(2):
                for b_ in range(2):
                    P.memset(OTt[h_][b_], 0.0, [], [("ot", h_, b_)], eng="pool")
            trps = [pbs[0][:, 0:64].bitcast(BF16), pbs[1][:, 0:64].bitcast(BF16)]
            qtiles = list(range(5)) if with_ctx else [1, 2, 3, 4]
            qlist = ([0, 1] if with_ctx else []) + list(range(2, 18))
            for c in range(8):
                P.barrier()
                for i in range(3):
                    P.dma(w3[i], wqkv[:, i * DM + c * 128:i * DM + (c + 1) * 128].rearrange("(k p) f -> p k f", p=128),
                          [], [("w3", i)], eng="pool", semkey="w3%d" % i)
                P.dma(bias, nab[2 * c:2 * c + 2].rearrange("h r k q -> k (h r) q"), [], ["bias"], eng="pool", semkey="nabias")

                def qk_stream(which):
                    dst, tl = (Qc, qtiles) if which == 0 else (Kc, list(range(5)))
                    pp, ssb = pbs[2 * which], pbs[2 * which + 1]
                    for ti in tl:
                        t0, N = TILES[ti]
                        for k in range(8):
                            P.mm(pp[:, 0:N], w3[which][:, k, :], u[:, k, t0:t0 + N], k == 0, k == 7, [("w3", which), ("u", k, ti)], [("pp", which)])
                        P.act(sqb[which][:, 0:N], pp[:, 0:N], AF.Square, [("pp", which)], [("sqb", which)])
                        P.mm(ssb[:, 0:N], blk_bf, sqb[which][:, 0:N], True, True, [("sqb", which), "cbf"], [("ssps", which)])
                        P.act(rt[which][:, 0:N], ssb[:, 0:N], AF.Ln, [("ssps", which)], [("rt", which)], scale=1.0 / 64, bias=EPS)
                        P.act(rstd[which][:, 0:N], rt[which][:, 0:N], AF.Exp, [("rt", which)], [("rstd", which)], scale=-0.5)
                        P.stt(dst[:, t0:t0 + N], pp[:, 0:N], gs[:, which:which + 1], rstd[which][:, 0:N], ALU.mult, ALU.mult,
                              [("pp", which), ("rstd", which), "gs"], [("qk", which, ti)])

                def v_stream():
                    for a in range(18):
                        ti = 0 if a < 2 else 1 + (a - 2) // 4
                        vp = pbs[4 + a % 2]
                        for k in range(8):
                            P.mm(vp[:, 0:128], u[:, k, a * 128:(a + 1) * 128], w3[2][:, k, :], k == 0, k == 7, [("u", k, ti), ("w3", 2)], [("vp", a % 2)])
                        P.cp(VA4[:, a, :, 0:64], vp[:, 0:128].rearrange("p (h e) -> p h e", h=2), [("vp", a % 2), "VAones"], [("VA", a)],
                             eng="pool" if False else "dve")

                P.interleave([lambda: qk_stream(0), lambda: qk_stream(1), v_stream])
                P.barrier()

                def att_stream(hh):
                    hs = slice(hh * 64, (hh + 1) * 64)
                    spb = (pbs[4], pbs[5]) if hh == 0 else (pbs[2], pbs[3])
                    op_ = pbs[6 + hh][:, 0:65]
                    npt = 0
                    for qi, qa in enumerate(qlist):
                        tiq = 0 if qa < 2 else 1 + (qa - 2) // 4
                        if qa < 2:
                            kts = [(0, None), (1, None)]
                        else:
                            m = qa - 2
                            so, sn = _NA_STRIP[m]
                            js = _NA_JS[m]
                            kts = [(2 + jj, so + i_) for i_, jj in enumerate(js)] + [(0, None), (1, None)]
                        groups = [kts[i:i + 4] for i in range(0, len(kts), 4)]
                        first = True
                        for gi, grp in enumerate(groups):
                            sp_ = spb[npt % 2]
                            pt = PT[hh][npt % 2]
                            pk = ("pt", hh, npt % 2)
                            spn = ("sp", hh, npt % 2)
                            nloc = sum(1 for (_, rl) in grp if rl is not None)
                            if nloc:
                                b0 = grp[0][1]
                                P.mm(sp_[:, 0:nloc * 128], ident_bf, bias[:, hh * NSTRIP_BLK + b0:hh * NSTRIP_BLK + b0 + nloc, :].rearrange("p r q -> p (r q)"),
                                     True, False, ["bias", "cbf"], [spn])
                            for si, (ka, rl) in enumerate(grp):
                                tik = 0 if ka < 2 else 1 + (ka - 2) // 4
                                P.mm(sp_[:, si * 128:(si + 1) * 128], Kc[hs, ka * 128:(ka + 1) * 128], Qc[hs, qa * 128:(qa + 1) * 128],
                                     rl is None, (rl is None) or (si == nloc - 1), [("qk", 1, tik), ("qk", 0, tiq)], [spn])
                            ng_ = len(grp)
                            P.act(pt[:, 0:ng_ * 128], sp_[:, 0:ng_ * 128], AF.Exp, [spn], [pk])
                            for si, (ka, rl) in enumerate(grp):
                                last = (gi == len(groups) - 1) and (si == ng_ - 1)
                                P.mm(op_, pt[:, si * 128:(si + 1) * 128], VA[:, ka, hh * 65:(hh + 1) * 65], first, last,
                                     [("VA", ka), pk], [("op", hh)])
                                first = False
                            npt += 1
                        ot = OTt[hh][qi % 2]
                        rc = rec[hh][qi % 2]
                        o2 = pbs[6 + hh]
                        P.recip(rc[:, 0:1], o2[:, 64:65], [("op", hh), ("rec", hh, qi % 2)], [("rec", hh, qi % 2)])
                        P.ts(ot[:, hs], o2[:, 0:64], rc[:, 0:1], None, ALU.mult, None,
                             [("op", hh), ("rec", hh, qi % 2), ("ot", hh, qi % 2)], [("ot", hh, qi % 2)])
                        tp_ = trps[hh]
                        P.tr(tp_, ot, ident_bf, [("ot", hh, qi % 2), "cbf"], [("trp", hh)])
                        P.cp(M[hs, c, qa * 128:(qa + 1) * 128], tp_[hs, :], [("trp", hh)], [("Mq", c, qa, hh)], eng="act")

                P.interleave([lambda: att_stream(0), lambda: att_stream(1)])
                for ti in range(5):
                    P.op("dve", lambda e: e.nop(), [("Mq", c, qa, h_) for qa in qlist for h_ in range(2)], [("M", c, ti)])
            if not with_ctx:
                for c in range(8):
                    P.memset(M[:, c, 0:NCTX], 0.0, [("M", c, 0)], [("M", c, 0)], eng="pool")

            if DEBUG_STOP == 'NA':
                return 'dumpM'
            P.barrier()
            out_proj(l, w_o)

        dump_m = False
        for l in layers:
            last = (l == DEPTH - 1)
            adaln(l)
            if mixer:
                modulate(l, 0, list(range(5)))
                if l % 2 == 0:
                    rv = even_mixer(l)
                else:
                    rv = na_mixer(l, not last)
                if rv == 'dumpM':
                    dump_m = True
                    break
            if ffn:
                tiles = [1, 2, 3, 4] if last else list(range(5))
                if l % 2 == 0:
                    dense_ffn(l, tiles)
                else:
                    moe_ffn(l, tiles)
        P.barrier()
        hout_v = hout.rearrange("(c p) t -> p c t", p=128)
        for c in range(8):
            if dump_m:
                P.dma(hout_v[:, c, :], M[:, c, :], [], [("hout", c)], eng="pool", semkey="hout")
            else:
                P.dma(hout_v[:, c, :], h[:, c, :], [("h", c, ti) for ti in range(5)], [("hout", c)], semkey="hout")
        P.op("sp", lambda e: e.nop(), [("hout", c) for c in range(8)], [])
        with nc.Block() as block:
            P.emit(block, st)
        build.last_stats = dict(n_ops={e: len(P.ops[e]) for e in ENGS}, n_sems=P.n_sems)
    return nc


WEIGHT_KEYS = ["w_mod", "ev_w_in", "ev_w_out", "rg_wa", "rg_wx", "na_w_qkv", "na_w_o",
               "ffn_w1", "ffn_w3", "ffn_w2", "moe_w1", "moe_w3", "moe_w2"]


def _needed_keys(layers, mixer=True, ffn=True):
    keys = ["w_mod"]
    for l in layers:
        if l % 2 == 0:
            if mixer:
                keys += ["ev_w_in", "ev_w_out", "rg_wa", "rg_wx"]
            if ffn:
                keys += ["ffn_w1", "ffn_w3", "ffn_w2"]
        else:
            if mixer:
                keys += ["na_w_qkv", "na_w_o", "nab"]
            if ffn:
                keys += ["moe_w1", "moe_w3", "moe_w2"]
    return sorted(set(keys))


def run_layers(inp, state, layers, cores=8, mixer=True, ffn=True, trace=False):
    offs = None
    vec_arrs = []
    for b in range(cores):
        off, arr = _vec_layout(inp, b)
        offs = off
        vec_arrs.append(arr)
    offs = dict(offs)
    offs["_n"] = vec_arrs[0].shape[1]
    nc = build(layers, offs, mixer=mixer, ffn=ffn)
    consts = _consts()
    keys = _needed_keys(layers, mixer, ffn)
    shared = {}
    for k in keys:
        if k == "nab":
            shared[k] = np.stack([_na_bias_strips(inp["na_rpb"][jj]) for jj in range(2)], 0)
        else:
            shared[k] = np.ascontiguousarray(np.asarray(inp[k], np.float32))
    in_maps = []
    for b in range(cores):
        m = {"xin": state[b], "vecs": vec_arrs[b], "consts": consts}
        m.update(shared)
        in_maps.append(m)
    res = run_bass_kernel_spmd(nc, in_maps, core_ids=list(range(cores)), trace=trace)
    run_layers.last = res
    return [np.asarray(r["hout"], np.float32) for r in res.results]


def _initial_state(inp, b):
    x = np.asarray(inp["x"][b], np.float32)
    ctx = np.asarray(inp["ctx"][b], np.float32)
    return np.ascontiguousarray(np.concatenate([ctx, x], axis=0).T)


LAUNCH_GROUPS = [[0, 1, 2, 3]]


def kernel(**inputs):
    state = [_initial_state(inputs, b) for b in range(8)]
    for grp in LAUNCH_GROUPS:
        state = run_layers(inputs, state, grp)
    out = np.stack([s[:, NCTX:].T for s in state], axis=0)
    return np.ascontiguousarray(out.astype(np.float32))
```
